# Optimizing a Trainium2 kernel written in Bass

```python
import math
import jax, jax.numpy as jnp
from jax import lax
import numpy as np

D_MODEL = 1024
BATCH = 4
SEQ = 8192
DEPTH = 2

N_HEADS = 8
HEAD_DIM = D_MODEL // N_HEADS
Q_LORA = 256
KV_LORA = 128
IDX_HEADS = 8
IDX_DIM = 64
IDX_TOPK_MAX = 256
N_KV_GROUPS = 2
HEADS_PER_GROUP = N_HEADS // N_KV_GROUPS
CMP_LEN = 32
CMP_STRIDE = 16
CMP_HID = 256
SLC_BLK = 64
N_SLC_MAX = 16
WIN = 512
D_FF = 4 * D_MODEL
REL_BUCKETS = 32
REL_MAX_DIST = 4096
Q_BLK = 128
EPS = 1e-6
NEG = -1e30
N_A_LAYERS = DEPTH // 2
N_B_LAYERS = DEPTH - N_A_LAYERS

kernel_name = 'yoco_dsa_nsa_hybrid'


def rmsnorm(x, g):
    xf = x.astype(jnp.float32)
    y = xf * lax.rsqrt(jnp.mean(xf * xf, axis=-1, keepdims=True) + EPS)
    return (y * g.astype(jnp.float32)).astype(x.dtype)


def masked_softmax(logits, mask):
    logits = jnp.where(mask, logits, NEG)
    m = jnp.max(logits, axis=-1, keepdims=True)
    e = jnp.where(mask, jnp.exp(logits - m), 0.0)
    return e / jnp.maximum(jnp.sum(e, axis=-1, keepdims=True), 1e-30)


def rel_bucket(dist):
    dist = jnp.maximum(dist, 0)
    exact = REL_BUCKETS // 2
    log_ratio = jnp.log(jnp.maximum(dist, 1).astype(jnp.float32) / exact) / math.log(REL_MAX_DIST / exact)
    large = exact + (log_ratio * (REL_BUCKETS - exact)).astype(jnp.int32)
    return jnp.where(dist < exact, dist, jnp.minimum(large, REL_BUCKETS - 1))


def gather_rows(table, idx):
    return table[idx]


def to_blocks(a):
    return jnp.moveaxis(a.reshape(a.shape[0], a.shape[1] // Q_BLK, Q_BLK, *a.shape[2:]), 1, 0)


def from_blocks(a):
    a = jnp.moveaxis(a, 0, 1)
    return a.reshape(a.shape[0], a.shape[1] * a.shape[2], *a.shape[3:])


def sq_relu_mlp(h, w_up, w_down):
    return jnp.square(jax.nn.relu(h @ w_up)) @ w_down


def dsa_mixer(h, rel_bias, w_in, g_q_lat, g_kv_lat, g_k_idx, w_uq, w_q_idx, w_uk, w_uv, w_o):
    B, T, _ = h.shape
    top_k = min(IDX_TOPK_MAX, T // 4)
    proj = h @ w_in
    c_q, c_kv, k_idx, w_idx = jnp.split(proj, [Q_LORA, Q_LORA + KV_LORA, Q_LORA + KV_LORA + IDX_DIM], axis=-1)
    c_q = rmsnorm(c_q, g_q_lat)
    c_kv = rmsnorm(c_kv, g_kv_lat)
    k_idx = rmsnorm(k_idx, g_k_idx)
    q = jnp.einsum('btr,rhd->bthd', c_q, w_uq)
    q_abs = jnp.einsum('bthd,chd->bthc', q, w_uk) * HEAD_DIM ** -0.5
    q_idx = jnp.einsum('btr,rhd->bthd', c_q, w_q_idx)
    w_idx = w_idx * (IDX_HEADS ** -0.5 * IDX_DIM ** -0.5)
    key_pos = jnp.arange(T, dtype=jnp.int32)

    def block(args):
        qa, qi, wi, t0 = args
        t = t0 + jnp.arange(Q_BLK, dtype=jnp.int32)
        rel = jax.nn.relu(jnp.einsum('bqhd,bsd->bqhs', qi, k_idx).astype(jnp.float32))
        score = jnp.einsum('bqh,bqhs->bqs', wi.astype(jnp.float32), rel)
        score = jnp.where(key_pos[None, None, :] <= t[None, :, None], score, NEG)
        _, sel = lax.top_k(score, top_k)
        c_sel = jax.vmap(gather_rows)(c_kv, sel)
        dist = t[None, :, None] - sel
        logits = jnp.einsum('bqhc,bqkc->bqhk', qa, c_sel).astype(jnp.float32)
        logits = logits + jnp.transpose(rel_bias[rel_bucket(dist)], (0, 1, 3, 2))
        p = masked_softmax(logits, (dist >= 0)[:, :, None, :])
        return jnp.einsum('bqhk,bqkc->bqhc', p.astype(c_sel.dtype), c_sel)

    t0s = jnp.arange(T // Q_BLK, dtype=jnp.int32) * Q_BLK
    o_lat = from_blocks(lax.map(block, (to_blocks(q_abs), to_blocks(q_idx), to_blocks(w_idx), t0s)))
    o = jnp.einsum('bthc,chd->bthd', o_lat, w_uv).reshape(B, T, N_HEADS * HEAD_DIM)
    return o @ w_o


def compress_blocks(raw, n_cmp, pos, w1, w2):
    B = raw.shape[0]
    idx = CMP_STRIDE * np.arange(n_cmp)[:, None] + np.arange(CMP_LEN)[None, :]
    blocks = raw[:, idx] + pos[None, None, :, None, :]
    flat = jnp.transpose(blocks, (0, 1, 3, 2, 4)).reshape(B, n_cmp, N_KV_GROUPS, CMP_LEN * HEAD_DIM)
    return jax.nn.gelu(flat @ w1) @ w2


def selection_map(n_cmp, n_slc):
    c0 = CMP_STRIDE * np.arange(n_cmp)[:, None]
    s0 = SLC_BLK * np.arange(n_slc)[None, :]
    ov = np.clip(np.minimum(c0 + CMP_LEN, s0 + SLC_BLK) - np.maximum(c0, s0), 0, None)
    return (ov / CMP_LEN).astype(np.float32)


def shared_kv(x, g_kv, w_kv, pos_k, pos_v, w1_k, w2_k, w1_v, w2_v):
    B, T, _ = x.shape
    hs = rmsnorm(x, g_kv)
    kv = (hs @ w_kv).reshape(B, T, 6, N_KV_GROUPS, HEAD_DIM)
    k_cmp, v_cmp, k_slc, v_slc, k_win, v_win = [kv[:, :, i] for i in range(6)]
    n_cmp = (T - CMP_LEN) // CMP_STRIDE + 1
    kc = compress_blocks(k_cmp, n_cmp, pos_k, w1_k, w2_k)
    vc = compress_blocks(v_cmp, n_cmp, pos_v, w1_v, w2_v)
    n_slc = T // SLC_BLK

    def to_sel(a):
        return jnp.transpose(a.reshape(B, n_slc, SLC_BLK, N_KV_GROUPS, HEAD_DIM), (0, 3, 1, 2, 4))

    pad = ((0, 0), (WIN, 0), (0, 0), (0, 0))
    return (kc, vc, to_sel(k_slc), to_sel(v_slc), jnp.pad(k_win, pad), jnp.pad(v_win, pad))


def nsa_mixer(h, rel_bias, kv_shared, w_in, w_o):
    kc, vc, ks_blk, vs_blk, kw_pad, vw_pad = kv_shared
    B, T, _ = h.shape
    G, R = N_KV_GROUPS, HEADS_PER_GROUP
    n_cmp = kc.shape[1]
    n_slc = ks_blk.shape[2]
    n_sel = min(N_SLC_MAX, n_slc)
    cmp_end = jnp.asarray(CMP_STRIDE * np.arange(n_cmp) + CMP_LEN - 1, dtype=jnp.int32)
    sel_map = jnp.asarray(selection_map(n_cmp, n_slc))
    blk_ids = jnp.arange(n_slc, dtype=jnp.int32)
    bias_gr = rel_bias.reshape(REL_BUCKETS, G, R)
    g_ids = jnp.arange(G, dtype=jnp.int32)[None, None, :, None]
    gather_groups = jax.vmap(jax.vmap(gather_rows, in_axes=(0, 1), out_axes=1))

    proj = h @ w_in
    q = proj[..., :N_HEADS * HEAD_DIM].reshape(B, T, G, R, HEAD_DIM) * HEAD_DIM ** -0.5
    gates = jax.nn.sigmoid(proj[..., N_HEADS * HEAD_DIM:].astype(jnp.float32)).astype(h.dtype).reshape(B, T, G, R, 3)

    def block(args):
        qb, gb, t0 = args
        t = t0 + jnp.arange(Q_BLK, dtype=jnp.int32)
        lc = jnp.einsum('bqgrd,bngd->bqgrn', qb, kc).astype(jnp.float32)
        pc = masked_softmax(lc, (cmp_end[None, :] <= t[:, None])[None, :, None, None, :])
        o_cmp = jnp.einsum('bqgrn,bngd->bqgrd', pc.astype(vc.dtype), vc)
        imp = jnp.einsum('bqgrn,nj->bqgj', pc, sel_map)
        cur = t[:, None] // SLC_BLK
        forced = (blk_ids[None, :] == 0) | (blk_ids[None, :] == cur) | (blk_ids[None, :] == cur - 1)
        future = blk_ids[None, :] * SLC_BLK > t[:, None]
        imp = jnp.where(forced[None, :, None, :], 1e9, imp)
        imp = jnp.where(future[None, :, None, :], NEG, imp)
        _, sel = lax.top_k(imp, n_sel)
        k_sel = gather_groups(ks_blk, sel).reshape(B, Q_BLK, G, n_sel * SLC_BLK, HEAD_DIM)
        v_sel = gather_groups(vs_blk, sel).reshape(B, Q_BLK, G, n_sel * SLC_BLK, HEAD_DIM)
        pos = (sel[..., None] * SLC_BLK + jnp.arange(SLC_BLK, dtype=jnp.int32)).reshape(B, Q_BLK, G, n_sel * SLC_BLK)
        dist_s = t[None, :, None, None] - pos
        bias_s = jnp.moveaxis(bias_gr[rel_bucket(dist_s), g_ids], -1, 3)
        ls = jnp.einsum('bqgrd,bqgkd->bqgrk', qb, k_sel).astype(jnp.float32) + bias_s
        ps = masked_softmax(ls, (dist_s >= 0)[:, :, :, None, :])
        o_slc = jnp.einsum('bqgrk,bqgkd->bqgrd', ps.astype(v_sel.dtype), v_sel)
        kw = lax.dynamic_slice_in_dim(kw_pad, t0, WIN + Q_BLK, axis=1)
        vw = lax.dynamic_slice_in_dim(vw_pad, t0, WIN + Q_BLK, axis=1)
        s_w = t0 - WIN + jnp.arange(WIN + Q_BLK, dtype=jnp.int32)
        dist_w = t[:, None] - s_w[None, :]
        wmask = (dist_w >= 0) & (dist_w < WIN) & (s_w[None, :] >= 0)
        bias_w = jnp.transpose(rel_bias[rel_bucket(dist_w)].reshape(Q_BLK, WIN + Q_BLK, G, R), (0, 2, 3, 1))
        lw = jnp.einsum('bqgrd,bsgd->bqgrs', qb, kw).astype(jnp.float32) + bias_w[None]
        pw = masked_softmax(lw, wmask[None, :, None, None, :])
        o_win = jnp.einsum('bqgrs,bsgd->bqgrd', pw.astype(vw.dtype), vw)
        return gb[..., 0:1] * o_cmp + gb[..., 1:2] * o_slc + gb[..., 2:3] * o_win

    t0s = jnp.arange(T // Q_BLK, dtype=jnp.int32) * Q_BLK
    o = from_blocks(lax.map(block, (to_blocks(q), to_blocks(gates), t0s)))
    return o.reshape(B, T, N_HEADS * HEAD_DIM) @ w_o


def setup_inputs(seed: int = 0) -> dict:
    key = jax.random.key(seed)
    ks = jax.random.split(key, 27)

    def nrm(k, shape, scale):
        return jax.random.normal(k, shape, jnp.float32) * scale

    def gain(k, shape):
        return 1.0 + 0.01 * jax.random.normal(k, shape, jnp.float32)

    hd = N_HEADS * HEAD_DIM
    return {
        'x': nrm(ks[0], (BATCH, SEQ, D_MODEL), 1.0),
        'g_attn': gain(ks[1], (DEPTH, D_MODEL)),
        'g_mlp': gain(ks[2], (DEPTH, D_MODEL)),
        'w_up': nrm(ks[3], (DEPTH, D_MODEL, D_FF), D_MODEL ** -0.5),
        'w_down': nrm(ks[4], (DEPTH, D_FF, D_MODEL), D_FF ** -0.5),
        'rel_bias': nrm(ks[5], (REL_BUCKETS, N_HEADS), 0.5),
        'a_w_in': nrm(ks[6], (N_A_LAYERS, D_MODEL, Q_LORA + KV_LORA + IDX_DIM + IDX_HEADS), D_MODEL ** -0.5),
        'a_g_q_lat': gain(ks[7], (N_A_LAYERS, Q_LORA)),
        'a_g_kv_lat': gain(ks[8], (N_A_LAYERS, KV_LORA)),
        'a_g_k_idx': gain(ks[9], (N_A_LAYERS, IDX_DIM)),
        'a_w_uq': nrm(ks[10], (N_A_LAYERS, Q_LORA, N_HEADS, HEAD_DIM), Q_LORA ** -0.5),
        'a_w_q_idx': nrm(ks[11], (N_A_LAYERS, Q_LORA, IDX_HEADS, IDX_DIM), Q_LORA ** -0.5),
        'a_w_uk': nrm(ks[12], (N_A_LAYERS, KV_LORA, N_HEADS, HEAD_DIM), KV_LORA ** -0.5),
        'a_w_uv': nrm(ks[13], (N_A_LAYERS, KV_LORA, N_HEADS, HEAD_DIM), KV_LORA ** -0.5),
        'a_w_o': nrm(ks[14], (N_A_LAYERS, hd, D_MODEL), hd ** -0.5),
        'g_kv_shared': gain(ks[15], (D_MODEL,)),
        'w_kv_shared': nrm(ks[16], (D_MODEL, 6 * N_KV_GROUPS * HEAD_DIM), D_MODEL ** -0.5),
        'cmp_pos_k': nrm(ks[17], (CMP_LEN, HEAD_DIM), 0.5),
        'cmp_pos_v': nrm(ks[18], (CMP_LEN, HEAD_DIM), 0.5),
        'cmp_w1_k': nrm(ks[19], (CMP_LEN * HEAD_DIM, CMP_HID), (CMP_LEN * HEAD_DIM) ** -0.5),
        'cmp_w2_k': nrm(ks[20], (CMP_HID, HEAD_DIM), CMP_HID ** -0.5),
        'cmp_w1_v': nrm(ks[21], (CMP_LEN * HEAD_DIM, CMP_HID), (CMP_LEN * HEAD_DIM) ** -0.5),
        'cmp_w2_v': nrm(ks[22], (CMP_HID, HEAD_DIM), CMP_HID ** -0.5),
        'b_w_in': nrm(ks[23], (N_B_LAYERS, D_MODEL, hd + 3 * N_HEADS), D_MODEL ** -0.5),
        'b_w_o': nrm(ks[24], (N_B_LAYERS, hd, D_MODEL), hd ** -0.5),
        'g_final': gain(ks[25], (D_MODEL,)),
    }


def reference(x, g_attn, g_mlp, w_up, w_down, rel_bias, a_w_in, a_g_q_lat, a_g_kv_lat, a_g_k_idx,
              a_w_uq, a_w_q_idx, a_w_uk, a_w_uv, a_w_o, g_kv_shared, w_kv_shared, cmp_pos_k, cmp_pos_v,
              cmp_w1_k, cmp_w2_k, cmp_w1_v, cmp_w2_v, b_w_in, b_w_o, g_final):
    kv_shared = None
    for l in range(DEPTH):
        h = rmsnorm(x, g_attn[l])
        if l < N_A_LAYERS:
            x = x + dsa_mixer(h, rel_bias, a_w_in[l], a_g_q_lat[l], a_g_kv_lat[l], a_g_k_idx[l],
                              a_w_uq[l], a_w_q_idx[l], a_w_uk[l], a_w_uv[l], a_w_o[l])
        else:
            j = l - N_A_LAYERS
            x = x + nsa_mixer(h, rel_bias, kv_shared, b_w_in[j], b_w_o[j])
        x = x + sq_relu_mlp(rmsnorm(x, g_mlp[l]), w_up[l], w_down[l])
        if l == N_A_LAYERS - 1:
            kv_shared = shared_kv(x, g_kv_shared, w_kv_shared, cmp_pos_k, cmp_pos_v,
                                  cmp_w1_k, cmp_w2_k, cmp_w1_v, cmp_w2_v)
    return rmsnorm(x, g_final)
```

```python
import math
from contextlib import ExitStack

import numpy as np
import ml_dtypes
import jax
import jax.numpy as jnp

import concourse.bass as bass
import concourse.mybir as mybir
from concourse.bass_utils import run_bass_kernel_spmd

F32 = mybir.dt.float32
BF16 = mybir.dt.bfloat16
U8 = mybir.dt.uint8
AF = mybir.ActivationFunctionType
ALU = mybir.AluOpType
AX = mybir.AxisListType

D = 1024
H = 8
HD = 128
QL = 256
KVL = 128
IDXD = 64
TOPK = 256
DFF = 4096
EPS = 1e-6
NEGB = -30000.0
NIT = 18
EPOCH = 30000
N_CORES = 8


class Tile:
    def __init__(self, h, name, is_dram=False):
        self.h = h.ap() if is_dram else h
        self.name = name
        self.w = None
        self.r = {}

    def __getitem__(self, idx):
        return self.h[idx]


class KB:
    def __init__(self, nc):
        self.nc = nc
        self.es = ExitStack()
        self.engs = {"pe": nc.tensor, "act": nc.scalar, "dve": nc.vector, "pool": nc.gpsimd, "sp": nc.sync}
        self.cnt = {e: 0 for e in self.engs}
        self.esems = {e: [] for e in self.engs}
        self.waited = {e: {} for e in self.engs}
        self.dma_sems = []
        self.dma_uses = []
        self.dma_rng = {"sp": (0, 28), "pool": (28, 40), "act": (28, 40)}
        self.dma_next = {"sp": 0, "pool": 28, "act": 28}
        self.uid = 0

    def sb(self, name, shape, dtype):
        self.uid += 1
        return Tile(self.es.enter_context(self.nc.sbuf_tensor(f"{name}_{self.uid}", list(shape), dtype)), name)

    def ps(self, name, shape, dtype=F32):
        self.uid += 1
        return Tile(self.es.enter_context(self.nc.psum_tensor(f"{name}_{self.uid}", list(shape), dtype)), name)

    def dram(self, name, shape, dtype, kind="Internal"):
        return Tile(self.nc.dram_tensor(name, list(shape), dtype, kind=kind), name, is_dram=True)

    def view(self, t, ap, name=None):
        n = Tile.__new__(Tile)
        n.h = ap
        n.name = name or t.name
        n.w = None
        n.r = {}
        return n

    def alias(self, t, name=None):
        n = Tile.__new__(Tile)
        n.h = t.h
        n.name = name or t.name
        n.w = None
        n.r = {}
        return n

    def _esem(self, e, epoch):
        lst = self.esems[e]
        while len(lst) <= epoch:
            lst.append(self.es.enter_context(self.nc.semaphore(f"s_{e}_{len(lst)}")))
        return lst[epoch]

    def _dsem(self, i):
        while len(self.dma_sems) <= i:
            self.dma_sems.append(self.es.enter_context(self.nc.semaphore(f"s_dma_{len(self.dma_sems)}")))
            self.dma_uses.append(0)
        return self.dma_sems[i]

    def _emit_wait(self, e, key, val):
        w = self.waited[e]
        if w.get(key, 0) >= val:
            return
        eng = self.engs[e]
        if key[0] == "dma":
            eng.wait_ge(self._dsem(key[1]), val)
        else:
            f = key[1]
            epoch, v = (val - 1) // EPOCH, (val - 1) % EPOCH + 1
            eng.wait_ge(self._esem(f, epoch), v)
        w[key] = val

    def _deps(self, e, reads, writes):
        need = {}

        def add(ev):
            if ev is None:
                return
            k, v = ev
            if need.get(k, 0) < v:
                need[k] = v

        for t in reads:
            add(t.w)
        for t in writes:
            add(t.w)
            for k, v in t.r.items():
                add((k, v))
        for k, v in need.items():
            if k == ("eng", "pe") and e == "pe":
                continue
            self._emit_wait(e, k, v)

    def _record(self, ev, reads, writes):
        k, v = ev
        for t in reads:
            if t.r.get(k, 0) < v:
                t.r[k] = v
        for t in writes:
            t.w = ev
            t.r = {}

    def op(self, e, fn, reads=(), writes=()):
        self._deps(e, reads, writes)
        ins = fn(self.engs[e])
        self.cnt[e] += 1
        seq = self.cnt[e]
        epoch = (seq - 1) // EPOCH
        ins.then_inc(self._esem(e, epoch), 1)
        self._record((("eng", e), seq), reads, writes)
        return ins

    def dma(self, q, out, in_, reads=(), writes=(), **kw):
        self._deps(q, reads, writes)
        lo_, hi_ = self.dma_rng[q]
        i = self.dma_next[q]
        self.dma_next[q] = lo_ + (i + 1 - lo_) % (hi_ - lo_)
        sem = self._dsem(i)
        key = ("dma", i)
        if self.dma_uses[i] > 0:
            self._emit_wait(q, key, 16 * self.dma_uses[i])
        self.dma_uses[i] += 1
        val = 16 * self.dma_uses[i]
        self.engs[q].dma_start(out=out, in_=in_, **kw).then_inc(sem, 16)
        self._record((key, val), reads, writes)

    def barrier(self):
        for e in self.engs:
            for i in range(len(self.dma_sems)):
                if self.dma_uses[i] > 0:
                    self._emit_wait(e, ("dma", i), 16 * self.dma_uses[i])
            for f in ("pe", "act", "dve", "pool"):
                if f != e and self.cnt[f] > 0:
                    self._emit_wait(e, ("eng", f), self.cnt[f])

    def finish(self):
        for i in range(len(self.dma_sems)):
            if self.dma_uses[i] > 0:
                self._emit_wait("sp", ("dma", i), 16 * self.dma_uses[i])
        for e in ("pe", "act", "dve", "pool"):
            if self.cnt[e] > 0:
                self._emit_wait("sp", ("eng", e), self.cnt[e])


def bc_ap(ap, dims):
    return bass.AP(tensor=ap.tensor, offset=ap.offset, ap=[list(ap.ap[0])] + [list(d) for d in dims])


class Ctx:
    pass


def make_ident(K, c):
    c.identf = K.sb("identf", [128, 128], F32)
    c.ident = K.sb("ident", [128, 128], BF16)
    K.op("pool", lambda e: e.iota(c.identf[:], pattern=[[1, 128]], base=0, channel_multiplier=-1,
                                  allow_small_or_imprecise_dtypes=True), writes=[c.identf])
    K.op("dve", lambda e: e.tensor_scalar(out=c.ident[:], in0=c.identf[:], scalar1=0.0, scalar2=None,
                                          op0=ALU.is_equal), reads=[c.identf], writes=[c.ident])


def cast_to_scratch(K, dst, src, nelem):
    C = 1024
    while nelem % C:
        C //= 2
    R = nelem // C
    dflat = dst.h.tensor.reshape([R, C]).ap() if hasattr(dst.h.tensor, "reshape") else None
    sflat = src.h.tensor.reshape([R, C]).ap()
    step = 4096
    for r0 in range(0, R, step):
        r1 = min(R, r0 + step)
        K.dma("pool", dflat[r0:r1, :], sflat[r0:r1, :], reads=[src], writes=[dst])


def load_bcast(K, dst, src_tile, n):
    src_ap = bass.AP(tensor=src_tile.h.tensor, offset=0, ap=[[0, 128], [1, n]])
    K.dma("sp", dst[:], src_ap, reads=[src_tile], writes=[dst])


def rmsnorm_rows(K, c, x_ap, xt, n, g_ap, gt, out_ap, ot, tag):
    ss = c.stat[tag + "_ss"]
    rs = c.stat[tag + "_rs"]
    K.op("act", lambda e: e.activation(out=c.junk_bf[:, 0:n], in_=x_ap, func=AF.Square, accum_out=ss[:]),
         reads=[xt], writes=[c.junk_bf, ss])
    K.op("act", lambda e: e.activation(out=rs[:], in_=ss[:], func=AF.Sqrt, scale=1.0 / n, bias=c.eps_t[:]),
         reads=[ss, c.eps_t], writes=[rs])
    K.op("dve", lambda e: e.reciprocal(out=rs[:], in_=rs[:]), reads=[rs], writes=[rs])
    K.op("dve", lambda e: e.scalar_tensor_tensor(out=out_ap, in0=x_ap, scalar=rs[:], in1=g_ap, op0=ALU.mult,
                                                 op1=ALU.mult), reads=[xt, rs, gt], writes=[ot])


def transposes(K, c, src_aps, src_tiles, pt, dst_ap, dst_tile, nparts_out, ncols_each, copy_eng="act"):
    pv = pt[:].bitcast(BF16)
    off = 0
    n = len(src_aps)
    for i, sap in enumerate(src_aps):
        rows = sap.shape[0]
        o = pv[0:nparts_out, off:off + rows]
        K.op("pe", lambda e, o=o, sap=sap, rows=rows: e.transpose(out=o, in_=sap, identity=c.ident[0:rows, 0:rows]),
             reads=list(src_tiles) + [c.ident], writes=[pt])
        off += rows
    srcv = pv[0:nparts_out, 0:off]
    if copy_eng == "act":
        K.op("act", lambda e: e.activation(out=dst_ap, in_=srcv, func=AF.Copy), reads=[pt], writes=[dst_tile])
    else:
        K.op("dve", lambda e: e.tensor_copy(out=dst_ap, in_=srcv), reads=[pt], writes=[dst_tile])


def attention(K, c, G, R, qT, sblocks, kT_dram, v_dram, tab, tab_of, maskfn, CH=4):
    nb = len(sblocks)
    chunks = [sblocks[i:i + CH] for i in range(0, nb, CH)]
    state = {"ci": -1}

    def load_chunk(ci):
        ch = chunks[ci]
        s0 = ch[0] * 128
        n = len(ch)
        kt = c.kTc[ci % 2]
        vt = c.vc[ci % 2]
        ksrc = bass.AP(tensor=kT_dram.h.tensor, offset=s0, ap=[[c.T, 128], [128 * c.T, G], [1, n * 128]])
        K.dma("sp", kt[:, :, 0:n * 128], ksrc, reads=[kT_dram], writes=[kt])
        vsrc = bass.AP(tensor=v_dram.h.tensor, offset=s0 * G * 130, ap=[[G * 130, 128], [128 * G * 130, n], [1, G * 130]])
        K.dma("sp", vt[:, 0:n, :], vsrc, reads=[v_dram], writes=[vt])

    def emit_qk(i):
        k = sblocks[i]
        ci, wi = divmod(i, CH)
        kt = c.kTc[ci % 2]
        L = c.PL[i % 2]
        to = tab_of(k)
        GH = G * R // 2
        for half in range(2):
            g = (half * GH) // R
            K.op("pe", lambda e, half=half, g=g: e.matmul(
                L[:, half * 512:(half + 1) * 512],
                lhsT=kt[:, g, wi * 128:(wi + 1) * 128],
                rhs=qT[:, half * 512:(half + 1) * 512], start=True, stop=False),
                 reads=[kt, qT], writes=[L])
            K.op("pe", lambda e, half=half: e.matmul(
                L[:, half * 512:(half + 1) * 512],
                lhsT=c.ident[:],
                rhs=tab[:, half * GH:(half + 1) * GH, to:to + 128], start=False, stop=True),
                 reads=[c.ident, tab], writes=[L])
        if maskfn is not None:
            state[("m", i)] = maskfn(k, i % 2)

    load_chunk(0)
    emit_qk(0)
    for i in range(nb):
        k = sblocks[i]
        ci, wi = divmod(i, CH)
        if wi == 0 and ci + 1 < len(chunks):
            load_chunk(ci + 1)
        if i + 1 < nb:
            emit_qk(i + 1)
        L = c.PL[i % 2]
        PT = c.PT[i % 2]
        K.op("act", lambda e: e.activation(out=PT[:], in_=L[:], func=AF.Exp), reads=[L], writes=[PT])
        if maskfn is not None:
            map_, mt = state.pop(("m", i))
            pv = PT[:].rearrange("p (g r q) -> p g r q", g=G, r=R)
            K.op("dve", lambda e: e.tensor_tensor(out=pv, in0=pv, in1=map_, op=ALU.mult), reads=[PT, mt], writes=[PT])
        vt = c.vc[ci % 2]
        for h in range(G * R):
            g = h // R
            bank, slot = divmod(h, 3)
            K.op("pe", lambda e, h=h, g=g, bank=bank, slot=slot: e.matmul(
                c.PO[:, bank, slot * 130:(slot + 1) * 130],
                lhsT=PT[:, h * 128:(h + 1) * 128],
                rhs=vt[:, wi, g * 130:(g + 1) * 130],
                start=(i == 0 and slot == 0), stop=(i == nb - 1), skip_group_check=True),
                 reads=[PT, vt], writes=[c.PO])


def attention_h(K, c, G, R, qT, sblocks, kT_dram, v_dram, tab, tab_of, maskfn, CH=4, mult_eng="dve", mask_dram=None):
    nb = len(sblocks)
    chunks = [sblocks[i:i + CH] for i in range(0, nb, CH)]
    masks = {}
    GH = G * R // 2

    def load_chunk(ci):
        ch = chunks[ci]
        s0 = ch[0] * 128
        n = len(ch)
        kt = c.kTc[ci % 2]
        vt = c.vc[ci % 2]
        ksrc = bass.AP(tensor=kT_dram.h.tensor, offset=s0, ap=[[c.T, 128], [128 * c.T, G], [1, n * 128]])
        K.dma("sp", kt[:, :, 0:n * 128], ksrc, reads=[kT_dram], writes=[kt])
        vsrc = bass.AP(tensor=v_dram.h.tensor, offset=s0 * G * 130, ap=[[G * 130, 128], [128 * G * 130, n], [1, G * 130]])
        K.dma("sp", vt[:, 0:n, :], vsrc, reads=[v_dram], writes=[vt])
        if mask_dram is not None:
            mt_ = c.mch[ci % 2]
            K.dma("sp", mt_[:, 0:n * 128], mask_dram.h[:, s0:s0 + n * 128], reads=[mask_dram], writes=[mt_])

    def emit_qk(i):
        k = sblocks[i]
        ci, wi = divmod(i, CH)
        kt = c.kTc[ci % 2]
        to = tab_of(k)
        for half in range(2):
            L = c.Lh[half]
            g = (half * GH) // R
            K.op("pe", lambda e, half=half, g=g, L=L: e.matmul(
                L[:, 0:512], lhsT=kt[:, g, wi * 128:(wi + 1) * 128],
                rhs=qT[:, half * 512:(half + 1) * 512], start=True, stop=False), reads=[kt, qT], writes=[L])
            K.op("pe", lambda e, half=half, L=L: e.matmul(
                L[:, 0:512], lhsT=c.ident[:],
                rhs=tab[:, half * GH:(half + 1) * GH, to:to + 128], start=False, stop=True), reads=[c.ident, tab], writes=[L])
        if maskfn is not None:
            if mask_dram is not None:
                masks[i] = maskfn(k, i % 2, c.mch[ci % 2], wi)
            else:
                masks[i] = maskfn(k, i % 2)

    load_chunk(0)
    emit_qk(0)
    for i in range(nb):
        ci, wi = divmod(i, CH)
        if wi == 0 and ci + 1 < len(chunks):
            load_chunk(ci + 1)
        PT = c.PT[i % 2]
        for half in range(2):
            L = c.Lh[half]
            K.op("act", lambda e, half=half, L=L: e.activation(out=PT[:, half * 512:(half + 1) * 512], in_=L[:, 0:512], func=AF.Exp),
                 reads=[L], writes=[PT])
        if i + 1 < nb:
            emit_qk(i + 1)
        if maskfn is not None:
            map_, mt = masks.pop(i)
            pv = PT[:].rearrange("p (g r q) -> p g r q", g=G, r=R)
            K.op(mult_eng, lambda e: e.tensor_tensor(out=pv, in0=pv, in1=map_, op=ALU.mult), reads=[PT, mt], writes=[PT])
        vt = c.vc[ci % 2]
        for h in range(G * R):
            g = h // R
            bank, slot = divmod(h, 3)
            K.op("pe", lambda e, h=h, g=g, bank=bank, slot=slot: e.matmul(
                c.PO[:, bank, slot * 130:(slot + 1) * 130],
                lhsT=PT[:, h * 128:(h + 1) * 128],
                rhs=vt[:, wi, g * 130:(g + 1) * 130],
                start=(i == 0 and slot == 0), stop=(i == nb - 1), skip_group_check=True),
                 reads=[PT, vt], writes=[c.PO])
        if i % 2 == 1 or i == nb - 1:
            yield 4.8


def attn_norm(K, c, nheads, out_fn, gate_fn=None):
    for bank in range(3):
        n = min(3, nheads - 3 * bank)
        if n <= 0:
            break
        zsrc = bc_ap(c.PO[:, bank, :], [[130, n], [1, 1]])
        zsrc = bass.AP(tensor=zsrc.tensor, offset=zsrc.offset + 128, ap=zsrc.ap)
        K.op("dve", lambda e, zsrc=zsrc, bank=bank, n=n: e.tensor_scalar(
            out=c.rz[:, 3 * bank:3 * bank + n].rearrange("p (n o) -> p n o", o=1), in0=zsrc, scalar1=1e-30, scalar2=None,
            op0=ALU.max), reads=[c.PO], writes=[c.rz])
    K.op("dve", lambda e: e.reciprocal(out=c.rz[:, 0:nheads], in_=c.rz[:, 0:nheads]), reads=[c.rz], writes=[c.rz])
    if gate_fn is not None:
        gate_fn()
    for h in range(nheads):
        bank, slot = divmod(h, 3)
        out_fn(h, c.PO[:, bank, slot * 130:slot * 130 + 128], c.rz[:, h:h + 1])


def mlp_phase(K, c, src_x, oT_dram, wo_bf, wup_bf, wdn_bf, g_mlp, out_dram, ntok, final_g=None, blend=None):
    TT = 256
    NSUB = TT // 128
    wo = K.sb("wo", [128, H, D], BF16)
    K.dma("sp", wo[:], wo_bf.h.rearrange("(h p) n -> p h n", p=128), reads=[wo_bf], writes=[wo])
    wdn = K.sb("wdn", [128, DFF // 128, D], BF16)
    for q in range(4):
        K.dma("sp", wdn[:, q * 8:(q + 1) * 8, :], wdn_bf.h[q * 1024:(q + 1) * 1024, :].rearrange("(c p) n -> p c n", p=128),
              reads=[wdn_bf], writes=[wdn])
    gm = K.sb("gm", [128, D], F32)
    load_bcast(K, gm, g_mlp, D)
    gf = None
    if final_g is not None:
        gf = K.sb("gf", [128, D], F32)
        load_bcast(K, gf, final_g, D)
    wup = [K.sb("wup", [128, 8, 512], BF16) for _ in range(2)]
    xt = [K.sb("xt", [128, NSUB, D], F32) for _ in range(2)]
    if blend is not None:
        xpp = [K.sb("xpp", [128, 2 * NSUB, D], F32) for _ in range(2)]
        psel = K.sb("pselm", [128, 2], F32)
        K.dma("sp", psel[:], blend.h, reads=[blend], writes=[psel])
    oT = [K.sb("oT", [128, H, TT], BF16) for _ in range(2)]
    hb = K.sb("hb", [128, NSUB, D], BF16)
    hT = K.sb("hT", [128, 8, TT], BF16)
    uT = K.sb("uT", [128, DFF // 128, TT], BF16)
    tmp = [K.sb("tmp", [128, TT], F32) for _ in range(2)]
    yo = [K.sb("yo", [128, D], F32) for _ in range(2)]
    wup_ctr = [0]

    def load_wup(fg):
        t = wup[wup_ctr[0] % 2]
        wup_ctr[0] += 1
        K.dma("sp", t[:], wup_bf.h[:, fg * 512:(fg + 1) * 512].rearrange("(k p) n -> p k n", p=128),
              reads=[wup_bf], writes=[t])
        return t

    ntiles = ntok // TT
    for ti in range(ntiles):
        x = xt[ti % 2]
        o = oT[ti % 2]
        if blend is None:
            K.dma("sp", x[:], src_x.h[ti * TT:(ti + 1) * TT, :].rearrange("(s p) n -> p s n", p=128), reads=[src_x], writes=[x])
        else:
            xp = xpp[ti % 2]
            K.dma("sp", xp[:], src_x.h[2 * ti * TT:2 * (ti + 1) * TT, :].rearrange("(s p) n -> p s n", p=128), reads=[src_x], writes=[xp])
            for s_ in range(NSUB):
                K.op("dve", lambda e, s_=s_: e.tensor_scalar(out=x[:, s_, :], in0=xp[:, 2 * s_, :], scalar1=psel[:, 0:1], scalar2=None, op0=ALU.mult),
                     reads=[xp, psel], writes=[x])
                K.op("dve", lambda e, s_=s_: e.scalar_tensor_tensor(out=x[:, s_, :], in0=xp[:, 2 * s_ + 1, :], scalar=psel[:, 1:2], in1=x[:, s_, :],
                                                                    op0=ALU.mult, op1=ALU.add), reads=[xp, psel, x], writes=[x])
        K.dma("sp", o[:], oT_dram.h[:, :, ti * TT:(ti + 1) * TT], reads=[oT_dram], writes=[o])
        for s in range(NSUB):
            P = c.PL[s % 2]
            for half in range(2):
                for h in range(H):
                    K.op("pe", lambda e, h=h, half=half, s=s, P=P: e.matmul(
                        P[:, half * 512:(half + 1) * 512], lhsT=o[:, h, s * 128:(s + 1) * 128],
                        rhs=wo[:, h, half * 512:(half + 1) * 512], start=(h == 0), stop=(h == H - 1)),
                         reads=[o, wo], writes=[P])
            K.op("dve", lambda e, s=s, P=P: e.tensor_tensor(out=x[:, s, :], in0=x[:, s, :], in1=P[:], op=ALU.add),
                 reads=[x, P], writes=[x])
            rmsnorm_rows(K, c, x[:, s, :], x, D, gm[:], gm, hb[:, s, :], hb, "m")
        for s in range(NSUB):
            transposes(K, c, [hb[:, s, k * 128:(k + 1) * 128] for k in range(8)], [hb], c.PM,
                       hT[:, :, s * 128:(s + 1) * 128], hT, 128, 128,
                       copy_eng="act" if s % 2 == 0 else "dve")
        nxt = load_wup(0)
        for fg in range(8):
            wt = nxt
            if fg + 1 < 8:
                nxt = load_wup(fg + 1)
            for fc in range(4):
                f = fg * 4 + fc
                P = c.PL[f % 2]
                for k in range(8):
                    K.op("pe", lambda e, k=k, fc=fc, P=P, wt=wt: e.matmul(
                        P[:, 0:TT], lhsT=wt[:, k, fc * 128:(fc + 1) * 128], rhs=hT[:, k, :],
                        start=(k == 0), stop=(k == 7)), reads=[wt, hT], writes=[P])
                tm = tmp[f % 2]
                K.op("act", lambda e, P=P, tm=tm: e.activation(out=tm[:], in_=P[:, 0:TT], func=AF.Relu),
                     reads=[P], writes=[tm])
                K.op("dve", lambda e, f=f, tm=tm: e.tensor_tensor(out=uT[:, f, :], in0=tm[:], in1=tm[:], op=ALU.mult),
                     reads=[tm], writes=[uT])
        for s in range(NSUB):
            y = yo[s % 2]
            for half in range(2):
                P = c.PL[half]
                for f in range(DFF // 128):
                    K.op("pe", lambda e, f=f, half=half, s=s, P=P: e.matmul(
                        P[:, 0:512], lhsT=uT[:, f, s * 128:(s + 1) * 128], rhs=wdn[:, f, half * 512:(half + 1) * 512],
                        start=(f == 0), stop=(f == DFF // 128 - 1)), reads=[uT, wdn], writes=[P])
                K.op("dve", lambda e, half=half, s=s, P=P, y=y: e.tensor_tensor(
                    out=y[:, half * 512:(half + 1) * 512], in0=x[:, s, half * 512:(half + 1) * 512], in1=P[:, 0:512],
                    op=ALU.add), reads=[x, P], writes=[y])
            if gf is not None:
                rmsnorm_rows(K, c, y[:], y, D, gf[:], gf, y[:], y, "m")
            r0 = ti * TT + s * 128
            K.dma("pool", out_dram.h[r0:r0 + 128, :], y[:], reads=[y], writes=[out_dram])


def common_setup(K, c, T, NQ):
    c.T = T
    c.NQ = NQ
    c.NS = T // 128
    make_ident(K, c)
    c.eps_t = K.sb("eps", [128, 1], F32)
    K.op("dve", lambda e: e.memset(c.eps_t[:], EPS), writes=[c.eps_t])
    c.junk_bf = K.sb("junk_bf", [128, D], BF16)
    c.stat = {}
    for tag in ("a", "b", "m", "n"):
        c.stat[tag + "_ss"] = K.sb("ss", [128, 1], F32)
        c.stat[tag + "_rs"] = K.sb("rs", [128, 1], F32)
    c.PL = [K.ps("PL0", [128, 1024]), K.ps("PL1", [128, 1024])]
    c.PO = K.ps("PO", [128, 3, 512])
    c.PM = K.ps("PM", [128, 512])


def build_A(T, debug=False):
    nc = bass.Bass("TRN2", target_bir_lowering=False)
    K = KB(nc)
    c = Ctx()
    with K.es:
        common_setup(K, c, T, T // 256)
        emit_A(K, c, T, "par", debug)
        K.finish()
    return nc


def emit_A(K, c, T, mode, debug=False):
    allm = mode == "all"
    NQ = T // 128 if allm else T // 256
    NS = T // 128
    NTAB = min(26, NS if allm else 2 * NQ)
    CW = 128 if allm else 256
    c.NTAB = NTAB
    ein = lambda n, s: K.dram(n, s, F32, kind="ExternalInput")
    xs = ein("xs", [T, D])
    xq = xs if allm else ein("xq", [NQ * 128, D])
    w_in = ein("a_w_in", [D, 456])
    g_attn = ein("g_attn0", [D])
    g_mlp = ein("g_mlp0", [D])
    g_q = ein("a_g_q_lat", [QL])
    g_kv = ein("a_g_kv_lat", [KVL])
    g_ki = ein("a_g_k_idx", [IDXD])
    w_uq = ein("a_w_uq", [QL, H * HD])
    w_qi = ein("a_w_q_idx", [QL, H * IDXD])
    w_uk = ein("a_w_uk", [KVL, H * HD])
    w_uv = ein("a_w_uv", [KVL, H * HD])
    w_o = ein("a_w_o", [D, D])
    w_up = ein("w_up0", [D, DFF])
    w_dn = ein("w_down0", [DFF, D])
    ftab_f = ein("ftabA", [128, H * NTAB * 128])
    cmask_d = ein("cmask", [128, CW])
    if allm:
        x1 = K.dram("x1_all", [T, D], F32)
    else:
        x1 = K.dram("x1", [NQ * 128, D], F32, kind="ExternalOutput")

    sc = lambda n, s, dt=BF16: K.dram("A_" + n, s, dt, kind="Internal")
    win_bf = sc("win_bf", [D, 456])
    wuq_bf = sc("wuq_bf", [QL, H * HD])
    wqi_bf = sc("wqi_bf", [QL, H * IDXD])
    wuk_bf = sc("wuk_bf", [KVL, H * HD])
    wuv_bf = sc("wuv_bf", [KVL, H * HD])
    wo_bf = sc("wo_bf", [D, D])
    wup_bf = sc("wup_bf", [D, DFF])
    wdn_bf = sc("wdn_bf", [DFF, D])
    ftab_bf = sc("ftab_bf", [128, H * NTAB * 128])
    ckvT_d = sc("ckvT_d", [1, 128, T])
    ckva_d = sc("ckva_d", [T, 130])
    kidxT_d = sc("kidxT_d", [IDXD, T])
    oT_d = sc("oT_d", [128, H, NQ * 128])

    if True:
        for dst, src, n in ((win_bf, w_in, D * 456), (wuq_bf, w_uq, QL * H * HD), (wqi_bf, w_qi, QL * H * IDXD),
                            (wuk_bf, w_uk, KVL * H * HD), (wuv_bf, w_uv, KVL * H * HD), (ftab_bf, ftab_f, 128 * H * NTAB * 128),
                            (wo_bf, w_o, D * D), (wup_bf, w_up, D * DFF), (wdn_bf, w_dn, DFF * D)):
            cast_to_scratch(K, dst, src, n)

        with ExitStack() as es1:
            saved = K.es
            K.es = es1
            ga = K.sb("ga", [128, D], F32)
            load_bcast(K, ga, g_attn, D)
            gkv = K.sb("gkv", [128, KVL], F32)
            load_bcast(K, gkv, g_kv, KVL)
            gki = K.sb("gki", [128, IDXD], F32)
            load_bcast(K, gki, g_ki, IDXD)
            wk = K.sb("wk", [128, 8, 192], BF16)
            K.dma("sp", wk[:], win_bf.h[:, 256:448].rearrange("(k p) n -> p k n", p=128), reads=[win_bf], writes=[wk])
            xb = [K.sb("xb", [128, D], F32) for _ in range(2)]
            hb = K.sb("hb1", [128, D], BF16)
            hT = K.sb("hT1", [128, 8, 128], BF16)
            ckv = [K.sb("ckv", [128, 130], BF16) for _ in range(2)]
            kix = [K.sb("kix", [128, IDXD], BF16) for _ in range(2)]
            cT = [K.sb("cT", [128, 128], BF16) for _ in range(2)]
            kT = [K.sb("kT", [IDXD, 128], BF16) for _ in range(2)]
            for t in ckv:
                K.op("dve", lambda e, t=t: e.memset(t[:, 128:129], 1.0), writes=[t])
                K.op("dve", lambda e, t=t: e.memset(t[:, 129:130], 0.0), writes=[t])
            hbs = [hb, K.sb("hb1b", [128, D], BF16)]
            hTs = [hT, K.sb("hT1b", [128, 8, 128], BF16)]

            def a1_stage1(i):
                x = xb[i % 2]
                K.dma("sp", x[:], xs.h[i * 128:(i + 1) * 128, :], reads=[xs], writes=[x])
                rmsnorm_rows(K, c, x[:], x, D, ga[:], ga, hbs[i % 2][:], hbs[i % 2], "n")
                transposes(K, c, [hbs[i % 2][:, k * 128:(k + 1) * 128] for k in range(8)], [hbs[i % 2]], c.PM, hTs[i % 2][:], hTs[i % 2], 128, 128)

            a1_stage1(0)
            for i in range(NS):
                if i + 1 < NS:
                    a1_stage1(i + 1)
                hT = hTs[i % 2]
                P = c.PL[i % 2]
                for k in range(8):
                    K.op("pe", lambda e, k=k, P=P: e.matmul(P[:, 0:192], lhsT=hT[:, k, :], rhs=wk[:, k, :],
                                                           start=(k == 0), stop=(k == 7)), reads=[hT, wk], writes=[P])
                cv = ckv[i % 2]
                ki = kix[i % 2]
                rmsnorm_rows(K, c, P[:, 0:128], P, KVL, gkv[:], gkv, cv[:, 0:128], cv, "a")
                rmsnorm_rows(K, c, P[:, 128:192], P, IDXD, gki[:], gki, ki[:], ki, "b")
                ct = cT[i % 2]
                kt = kT[i % 2]
                transposes(K, c, [cv[:, 0:128]], [cv], c.PM, ct[:], ct, 128, 128, copy_eng="dve")
                transposes(K, c, [ki[:, :]], [ki], c.PM, kt[:], kt, IDXD, 128, copy_eng="act")
                K.dma("pool", ckva_d.h[i * 128:(i + 1) * 128, :], cv[:], reads=[cv], writes=[ckva_d])
                K.dma("pool", ckvT_d.h[0, :, i * 128:(i + 1) * 128], ct[:], reads=[ct], writes=[ckvT_d])
                K.dma("pool", kidxT_d.h[:, i * 128:(i + 1) * 128], kt[:], reads=[kt], writes=[kidxT_d])
            K.barrier()
            K.es = saved

        with ExitStack() as es2:
            saved = K.es
            K.es = es2
            ga = K.sb("ga", [128, D], F32)
            load_bcast(K, ga, g_attn, D)
            gq = K.sb("gq", [128, QL], F32)
            load_bcast(K, gq, g_q, QL)
            wq = K.sb("wq", [128, 8, 264], BF16)
            K.dma("sp", wq[:, :, 0:256], win_bf.h[:, 0:256].rearrange("(k p) n -> p k n", p=128), reads=[win_bf], writes=[wq])
            K.dma("sp", wq[:, :, 256:264], win_bf.h[:, 448:456].rearrange("(k p) n -> p k n", p=128), reads=[win_bf], writes=[wq])
            wuq = K.sb("wuq", [128, 2, H * HD], BF16)
            K.dma("sp", wuq[:], wuq_bf.h.rearrange("(k p) n -> p k n", p=128), reads=[wuq_bf], writes=[wuq])
            wqi = K.sb("wqi", [128, 2, H * IDXD], BF16)
            K.dma("sp", wqi[:], wqi_bf.h.rearrange("(k p) n -> p k n", p=128), reads=[wqi_bf], writes=[wqi])
            wuk = K.sb("wuk", [128, H * HD], BF16)
            K.dma("sp", wuk[:], wuk_bf.h, reads=[wuk_bf], writes=[wuk])
            wuv = K.sb("wuv", [128, H * HD], BF16)
            K.dma("sp", wuv[:], wuv_bf.h, reads=[wuv_bf], writes=[wuv])
            wukT = K.sb("wukT", [128, H, 128], BF16)
            transposes(K, c, [wuk[:, h * 128:(h + 1) * 128] for h in range(H)], [wuk], c.PM, wukT[:], wukT, 128, 128)
            ftab = K.sb("ftab", [128, H, NTAB * 128], BF16)
            K.dma("sp", ftab[:], ftab_bf.h.rearrange("p (h n) -> p h n", h=H), reads=[ftab_bf], writes=[ftab])
            cmask = K.sb("cmask", [128, CW], F32)
            K.dma("sp", cmask[:], cmask_d.h, reads=[cmask_d], writes=[cmask])
            scores = [K.sb("score", [128, T], F32) for _ in range(2)]
            mstage = K.sb("mstage", [128, T], BF16)
            maskD = [K.dram("A_maskD%d" % i, [128, T], BF16) for i in range(2)]
            c.mch = [K.sb("mch", [128, 512], BF16) for _ in range(2)]
            xb = [K.sb("xb", [128, D], F32) for _ in range(1)]
            hb = K.sb("hb2", [128, D], BF16)
            hT = K.sb("hT2", [128, 8, 128], BF16)
            cq = K.sb("cq", [128, QL], BF16)
            cqT = K.sb("cqT", [128, 2, 128], BF16)
            wi = K.sb("wi", [128, 8], F32)
            qTs = K.sb("qTs", [128, H, 128], BF16)
            qaTs = [K.sb("qaT", [128, H * 128], BF16) for _ in range(3)]
            qiT = K.sb("qiT", [IDXD, 128, H], BF16)
            Amat = K.sb("Amat", [128, 128, H], BF16)
            Wg = K.sb("Wg", [128, 8, 128], BF16)
            Rsb = [K.sb("Rsb", [128, 512], BF16) for _ in range(2)]
            kidc = [K.sb("kidc", [IDXD, 512], BF16) for _ in range(2)]
            c.kTc = [K.sb("kTc", [128, 1, 512], BF16) for _ in range(2)]
            c.vc = [K.sb("vc", [128, 4, 130], BF16) for _ in range(2)]
            c.PT = [K.sb("PT", [128, 1024], BF16) for _ in range(2)]
            c.rz = K.sb("rz", [128, 8], F32)
            lo = K.sb("lo", [128, 1], F32)
            hi = K.sb("hi", [128, 1], F32)
            mid = K.sb("mid", [128, 1], F32)
            cnt = K.sb("cnt", [128, 1], F32)
            tsel = K.sb("tsel", [128, 1], F32)
            thrs = [K.sb("thr", [128, 1], F32) for _ in range(2)]
            wtab = K.sb("wtab", [128, NIT + 1], F32)
            p2 = K.sb("p2", [128, NIT + 1], F32)
            for k in range(NIT + 1):
                K.op("dve", lambda e, k=k: e.memset(p2[:, k:k + 1], 2.0 ** (-(k + 1))), writes=[p2])
            mk = [K.sb("mk", [128, 128], BF16) for _ in range(2)]
            mkT = [K.sb("mkT", [128, 128], BF16) for _ in range(2)]
            olat = K.sb("olat", [128, H, 128], BF16)
            olatT = K.sb("olatT", [128, H, 128], BF16)
            oTs = K.sb("oTs", [128, H, 128], BF16)
            c.Lh = [K.view(c.PL[0], c.PL[0][:, 0:512], "L0"), K.view(c.PL[0], c.PL[0][:, 512:1024], "L1")]
            PF = K.view(c.PL[1], c.PL[1][:, 0:512], "PF")
            PSC = K.view(c.PL[1], c.PL[1][:, 512:1024], "PSC")

            def nsb_of(j):
                return (j + 1) if allm else (2 * j + 2)

            def f1(j):
                score, qaT = scores[j % 2], qaTs[j % 3]
                x = xb[0]
                K.dma("sp", x[:], xq.h[j * 128:(j + 1) * 128, :], reads=[xq], writes=[x])
                rmsnorm_rows(K, c, x[:], x, D, ga[:], ga, hb[:], hb, "a")
                transposes(K, c, [hb[:, k * 128:(k + 1) * 128] for k in range(8)], [hb], PF, hT[:], hT, 128, 128)
                yield 3.0
                for k in range(8):
                    K.op("pe", lambda e, k=k: e.matmul(PSC[:, 0:264], lhsT=hT[:, k, :], rhs=wq[:, k, :],
                                                      start=(k == 0), stop=(k == 7)), reads=[hT, wq], writes=[PSC])
                rmsnorm_rows(K, c, PSC[:, 0:256], PSC, QL, gq[:], gq, cq[:], cq, "b")
                K.op("act", lambda e: e.activation(out=wi[:], in_=PSC[:, 256:264], func=AF.Copy,
                                                   scale=float(8 ** -0.5 * IDXD ** -0.5)), reads=[PSC], writes=[wi])
                transposes(K, c, [cq[:, 0:128], cq[:, 128:256]], [cq], PF, cqT[:], cqT, 128, 128)
                yield 3.0
                for hf in range(2):
                    P = PF if hf == 0 else PSC
                    for hh in range(4):
                        h = hf * 4 + hh
                        for rc in range(2):
                            K.op("pe", lambda e, h=h, hh=hh, rc=rc, P=P: e.matmul(P[:, hh * 128:(hh + 1) * 128],
                                                                                lhsT=wuq[:, rc, h * 128:(h + 1) * 128], rhs=cqT[:, rc, :],
                                                                                start=(rc == 0), stop=(rc == 1)), reads=[wuq, cqT], writes=[P])
                    K.op("act", lambda e, hf=hf, P=P: e.activation(out=qTs[:, hf * 4:(hf + 1) * 4, :], in_=P[:, 0:512], func=AF.Copy),
                         reads=[P], writes=[qTs])
                yield 2.0
                for hf in range(2):
                    P = PF if hf == 0 else PSC
                    for hh in range(4):
                        h = hf * 4 + hh
                        K.op("pe", lambda e, h=h, hh=hh, P=P: e.matmul(P[:, hh * 128:(hh + 1) * 128], lhsT=wukT[:, h, :], rhs=qTs[:, h, :],
                                                                     start=True, stop=True), reads=[wukT, qTs], writes=[P])
                    K.op("dve", lambda e, hf=hf, P=P: e.tensor_scalar(out=qaT[:, hf * 512:(hf + 1) * 512], in0=P[:, 0:512], scalar1=float(HD ** -0.5),
                                                                     scalar2=None, op0=ALU.mult), reads=[P], writes=[qaT])
                yield 2.0
                for hf in range(2):
                    P = PF if hf == 0 else PSC
                    for hh in range(4):
                        h = hf * 4 + hh
                        for rc in range(2):
                            K.op("pe", lambda e, h=h, hh=hh, rc=rc, P=P: e.matmul(P[0:IDXD, hh * 128:(hh + 1) * 128],
                                                                                lhsT=wqi[:, rc, h * IDXD:(h + 1) * IDXD], rhs=cqT[:, rc, :],
                                                                                start=(rc == 0), stop=(rc == 1)), reads=[wqi, cqT], writes=[P])
                    K.op("act", lambda e, hf=hf, P=P: e.activation(out=qiT[:, :, hf * 4:(hf + 1) * 4].rearrange("p t h -> p h t"),
                                                                   in_=P[0:IDXD, 0:512].rearrange("p (h t) -> p h t", h=4), func=AF.Copy),
                         reads=[P], writes=[qiT])
                yield 2.0
                K.op("dve", lambda e: e.tensor_tensor(
                    out=Amat[:], in0=c.ident[:].rearrange("p (t o) -> p t o", o=1).to_broadcast([128, 128, H]),
                    in1=wi[:].rearrange("p (o h) -> p o h", o=1).to_broadcast([128, 128, H]), op=ALU.mult),
                     reads=[c.ident, wi], writes=[Amat])
                transposes(K, c, [Amat[:, g * 16:(g + 1) * 16, :].rearrange("p t h -> p (t h)") for g in range(8)], [Amat],
                           PF, Wg[:], Wg, 128, 128, copy_eng="dve")
                yield 3.0
                S = nsb_of(j) * 128
                nch = (S + 511) // 512
                for sc_ in range(nch):
                    n = min(512, S - sc_ * 512)
                    kc_ = kidc[sc_ % 2]
                    K.dma("sp", kc_[:, 0:n], kidxT_d.h[:, sc_ * 512:sc_ * 512 + n], reads=[kidxT_d], writes=[kc_])
                    for g in range(8):
                        K.op("pe", lambda e, g=g: e.matmul(PF[:, 0:n], lhsT=qiT[:, g * 16:(g + 1) * 16, :].rearrange("p t h -> p (t h)"),
                                                          rhs=kc_[:, 0:n], start=True, stop=True), reads=[qiT, kc_], writes=[PF])
                        Rt = Rsb[g % 2]
                        K.op("act", lambda e: e.activation(out=Rt[:, 0:n], in_=PF[:, 0:n], func=AF.Relu), reads=[PF], writes=[Rt])
                        K.op("pe", lambda e, g=g: e.matmul(PSC[:, 0:n], lhsT=Wg[:, g, :], rhs=Rt[:, 0:n],
                                                          start=(g == 0), stop=(g == 7)), reads=[Wg, Rt], writes=[PSC])
                        if g % 4 == 3:
                            yield 4.0 * n / 512
                    K.op("act", lambda e: e.activation(out=score[:, sc_ * 512:sc_ * 512 + n], in_=PSC[:, 0:n], func=AF.Copy),
                         reads=[PSC], writes=[score])

            def f1_cost(j):
                S = nsb_of(j) * 128
                return 15.0 + 2.0 * S / 512 * 4

            def f2(j):
                score, thr, msk = scores[j % 2], thrs[j % 2], mstage
                S = nsb_of(j) * 128
                if S <= TOPK:
                    K.op("dve", lambda e: e.tensor_tensor(out=score[:, S - CW:S], in0=score[:, S - CW:S], in1=cmask[:],
                                                          op=ALU.add), reads=[score, cmask], writes=[score])
                    K.op("dve", lambda e: e.memset(thr[:], -1e29), writes=[thr])
                    yield 1.0
                else:
                    K.op("dve", lambda e: e.tensor_reduce(out=lo[:], in_=score[:, 0:S], axis=AX.X, op=ALU.min),
                         reads=[score], writes=[lo])
                    K.op("dve", lambda e: e.tensor_tensor(out=score[:, S - CW:S], in0=score[:, S - CW:S], in1=cmask[:],
                                                          op=ALU.add), reads=[score, cmask], writes=[score])
                    yield S / 960.0
                    K.op("dve", lambda e: e.tensor_reduce(out=hi[:], in_=score[:, 0:S], axis=AX.X, op=ALU.max),
                         reads=[score], writes=[hi])
                    K.op("dve", lambda e: e.tensor_tensor(out=hi[:], in0=hi[:], in1=lo[:], op=ALU.subtract),
                         reads=[hi, lo], writes=[hi])
                    K.op("dve", lambda e: e.tensor_scalar(out=wtab[:], in0=p2[:], scalar1=hi[:], scalar2=None, op0=ALU.mult),
                         reads=[p2, hi], writes=[wtab])
                    K.op("dve", lambda e: e.tensor_tensor(out=mid[:], in0=lo[:], in1=wtab[:, 0:1], op=ALU.add),
                         reads=[lo, wtab], writes=[mid])
                    yield S / 960.0
                    for it in range(NIT):
                        K.op("dve", lambda e: e.tensor_scalar(out=msk[:, 0:S], in0=score[:, 0:S], scalar1=mid[:], scalar2=None,
                                                              op0=ALU.is_ge, op1=ALU.add, accum_out=cnt[:]),
                             reads=[score, mid], writes=[msk, cnt])
                        K.op("dve", lambda e: e.tensor_scalar(out=tsel[:], in0=cnt[:], scalar1=float(TOPK), scalar2=0.5,
                                                              op0=ALU.is_ge, op1=ALU.subtract), reads=[cnt], writes=[tsel])
                        K.op("dve", lambda e, it=it: e.scalar_tensor_tensor(out=mid[:], in0=tsel[:], scalar=wtab[:, it:it + 1],
                                                                            in1=mid[:], op0=ALU.mult, op1=ALU.add),
                             reads=[tsel, wtab, mid], writes=[mid])
                        yield S / 960.0 + 0.5
                    K.op("dve", lambda e: e.tensor_tensor(out=thr[:], in0=mid[:], in1=wtab[:, NIT:NIT + 1], op=ALU.subtract),
                         reads=[mid, wtab], writes=[thr])
                K.op("dve", lambda e: e.tensor_scalar(out=msk[:, 0:S], in0=score[:, 0:S], scalar1=thr[:], scalar2=None, op0=ALU.is_ge),
                     reads=[score, thr], writes=[msk])
                md = maskD[j % 2]
                K.dma("pool", md.h[:, 0:S], msk[:, 0:S], reads=[msk], writes=[md])
                yield S / 960.0

            def f2_cost(j):
                S = nsb_of(j) * 128
                return (NIT + 3) * (S / 960.0 + 0.5)

            def back(j):
                qaT = qaTs[j % 3]

                def maskfn(k, slot, mchunk, wi):
                    pm = c.PM[:].bitcast(BF16)[:, slot * 128:(slot + 1) * 128]
                    K.op("pe", lambda e: e.transpose(out=pm, in_=mchunk[:, wi * 128:(wi + 1) * 128], identity=c.ident[:]),
                         reads=[mchunk, c.ident], writes=[c.PM])
                    mt = mkT[slot]
                    K.op("act", lambda e: e.activation(out=mt[:], in_=pm, func=AF.Copy), reads=[c.PM], writes=[mt])
                    return mt[:].rearrange("p (g o q) -> p g o q", g=1, o=1).to_broadcast([128, 1, H, 128]), mt

                if allm:
                    tab_of = lambda k: min(j - k, NTAB - 1) * 128
                else:
                    tab_of = lambda k: min(2 * j - k + 1, NTAB - 1) * 128
                yield from attention_h(K, c, 1, H, qaT, list(range(nsb_of(j))), ckvT_d, ckva_d, ftab, tab_of, maskfn, mult_eng="pool",
                                       mask_dram=maskD[j % 2])

                def put(h, o_ap, rz_ap):
                    if h % 2 == 0:
                        K.op("act", lambda e: e.activation(out=olat[:, h, :], in_=o_ap, func=AF.Copy, scale=rz_ap),
                             reads=[c.PO, c.rz], writes=[olat])
                    else:
                        K.op("dve", lambda e: e.tensor_scalar(out=olat[:, h, :], in0=o_ap, scalar1=rz_ap, scalar2=None,
                                                              op0=ALU.mult), reads=[c.PO, c.rz], writes=[olat])
                attn_norm(K, c, H, put)
                yield 4.0
                transposes(K, c, [olat[:, h, :] for h in range(H)], [olat], c.Lh[0], olatT[:], olatT, 128, 128)
                for hf in range(2):
                    P = c.Lh[1] if hf == 0 else c.Lh[0]
                    for hh in range(4):
                        h = hf * 4 + hh
                        K.op("pe", lambda e, h=h, hh=hh, P=P: e.matmul(P[:, hh * 128:(hh + 1) * 128], lhsT=wuv[:, h * 128:(h + 1) * 128],
                                                                     rhs=olatT[:, h, :], start=True, stop=True), reads=[wuv, olatT], writes=[P])
                    K.op("act", lambda e, hf=hf, P=P: e.activation(out=oTs[:, hf * 4:(hf + 1) * 4, :], in_=P[:, 0:512], func=AF.Copy),
                         reads=[P], writes=[oTs])
                K.dma("pool", oT_d.h[:, :, j * 128:(j + 1) * 128], oTs[:], reads=[oTs], writes=[oT_d])
                yield 4.0

            def back_cost(j):
                return 2.4 * nsb_of(j) + 8.0

            def run_all(g):
                for _ in g:
                    pass

            for t in range(NQ + 2):
                streams = []
                if t < NQ:
                    streams.append([f1(t), f1_cost(t), 0.0])
                if 0 <= t - 1 < NQ:
                    streams.append([f2(t - 1), f2_cost(t - 1), 0.0])
                if 0 <= t - 2 < NQ:
                    streams.append([back(t - 2), back_cost(t - 2), 0.0])
                while streams:
                    st = min(streams, key=lambda s_: s_[2] / s_[1])
                    try:
                        st[2] += next(st[0])
                    except StopIteration:
                        streams.remove(st)
            K.barrier()
            K.es = saved

        with ExitStack() as es3:
            saved = K.es
            K.es = es3
            mlp_phase(K, c, xq, oT_d, wo_bf, wup_bf, wdn_bf, g_mlp, x1, NQ * 128)
            if debug:
                for nm, t, shp in (("ckva", ckva_d, [T, 130]), ("ckvT", ckvT_d, [128, T]), ("kidxT", kidxT_d, [IDXD, T]),
                                   ("oT", oT_d, [128 * H, NQ * 128])):
                    o = K.dram("dbg_" + nm, shp, F32, kind="ExternalOutput")
                    K.dma("pool", o.h, t.h.tensor.reshape(shp).ap(), reads=[t], writes=[o])
            K.barrier()
            K.es = saved
    return x1


CMP_LEN = 32
CMP_STRIDE = 16
CMP_HID = 256
N_SEL = 16


def build_B(T, debug=False):
    nc = bass.Bass("TRN2", target_bir_lowering=False)
    K = KB(nc)
    c = Ctx()
    with K.es:
        common_setup(K, c, T, T // 256)
        emit_B(K, c, T, None, debug)
        K.finish()
    return nc


def build_F(T):
    nc = bass.Bass("TRN2", target_bir_lowering=False)
    K = KB(nc)
    c = Ctx()
    with K.es:
        common_setup(K, c, T, T // 256)
        x1_all = emit_A(K, c, T, "all")
        emit_B(K, c, T, x1_all)
        K.finish()
    return nc


def emit_B(K, c, T, x1_src=None, debug=False):
    fused = x1_src is not None
    NQ = T // 256
    NS = T // 128
    NTAB = min(26, 2 * NQ)
    NB = T // 64
    NCP = T // 16
    n_cmp = NCP - 1
    NCC = NCP // 128
    ein = lambda n, s: K.dram(n, s, F32, kind="ExternalInput")
    if fused:
        xs = x1_src
        xq = None
        psel_d = ein("psel", [128, 2])
    else:
        xs = ein("xs", [T, D])
        xq = ein("xq", [NQ * 128, D])
    g_kvs = ein("g_kv_shared", [D])
    w_kvs = ein("w_kv_shared", [D, 1536])
    pos_k = ein("cmp_pos_k", [CMP_LEN, HD])
    pos_v = ein("cmp_pos_v", [CMP_LEN, HD])
    w1k = ein("cmp_w1_k", [CMP_LEN * HD, CMP_HID])
    w1v = ein("cmp_w1_v", [CMP_LEN * HD, CMP_HID])
    w2k = ein("cmp_w2_k", [CMP_HID, HD])
    w2v = ein("cmp_w2_v", [CMP_HID, HD])
    b_w_in = ein("b_w_in", [D, 1048])
    b_w_o = ein("b_w_o", [D, D])
    g_attn = ein("g_attn1", [D])
    g_mlp = ein("g_mlp1", [D])
    g_fin = ein("g_final", [D])
    w_up = ein("w_up1", [D, DFF])
    w_dn = ein("w_down1", [DFF, D])
    ftab_f = ein("ftabB", [128, H * NTAB * 128])
    wtab_f = ein("wtab", [128, H * 6 * 128])
    cm01_f = ein("cm01", [NQ * 128, NCC * 128])
    keep_d = ein("keep", [NQ * 128, NB])
    add_d = ein("addm", [NQ * 128, NB])
    selmap_f = ein("selmap", [NCP, NB])
    eall_f = ein("eall", [128, T])
    out = K.dram("out", [NQ * 128, D], F32, kind="ExternalOutput")

    sc = lambda n, s, dt=BF16: K.dram("B_" + n, s, dt)
    wkv_bf = sc("wkv_bf", [D, 1536])
    w1k_bf = sc("w1k_bf", [CMP_LEN * HD, CMP_HID])
    w1v_bf = sc("w1v_bf", [CMP_LEN * HD, CMP_HID])
    w2k_bf = sc("w2k_bf", [CMP_HID, HD])
    w2v_bf = sc("w2v_bf", [CMP_HID, HD])
    posk_bf = sc("posk_bf", [CMP_LEN, HD])
    posv_bf = sc("posv_bf", [CMP_LEN, HD])
    win_bf = sc("bwin_bf", [D, 1048])
    wo_bf = sc("wo_bf", [D, D])
    wup_bf = sc("wup_bf", [D, DFF])
    wdn_bf = sc("wdn_bf", [DFF, D])
    ftab_bf = sc("ftab_bf", [128, H * NTAB * 128])
    wtab_bf = sc("wtab_bf", [128, H * 6 * 128])
    cm01_bf = sc("cm01_bf", [NQ * 128, NCC * 128])
    selmap_bf = sc("selmap_bf", [NCP, NB])
    eall_bf = sc("eall_bf", [128, T])
    kslcT_d = sc("kslcT_d", [2, 128, T])
    kwinT_d = sc("kwinT_d", [2, 128, T])
    vslc_d = sc("vslc_d", [T, 2 * 130])
    vwin_d = sc("vwin_d", [T, 2 * 130])
    kcT_d = sc("kcT_d", [128, 2 * NCP])
    vca_d = sc("vca_d", [128, NCC * 2 * 130])
    oT_d = sc("oT_d", [128, H, NQ * 128])

    if True:
        for dst, src, n in ((wkv_bf, w_kvs, D * 1536), (w1k_bf, w1k, 4096 * 256), (w1v_bf, w1v, 4096 * 256),
                            (w2k_bf, w2k, 256 * 128), (w2v_bf, w2v, 256 * 128), (posk_bf, pos_k, 32 * 128), (posv_bf, pos_v, 32 * 128),
                            (win_bf, b_w_in, D * 1048), (ftab_bf, ftab_f, 128 * H * NTAB * 128), (wtab_bf, wtab_f, 128 * H * 6 * 128),
                            (cm01_bf, cm01_f, NQ * 128 * NCC * 128), (selmap_bf, selmap_f, NCP * NB), (eall_bf, eall_f, 128 * T),
                            (wo_bf, b_w_o, D * D), (wup_bf, w_up, D * DFF), (wdn_bf, w_dn, DFF * D)):
            cast_to_scratch(K, dst, src, n)

        with ExitStack() as es1:
            saved = K.es
            K.es = es1
            gk = K.sb("gk", [128, D], F32)
            load_bcast(K, gk, g_kvs, D)
            wkv = K.sb("wkv", [128, 8, 1536], BF16)
            K.dma("sp", wkv[:], wkv_bf.h.rearrange("(k p) n -> p k n", p=128), reads=[wkv_bf], writes=[wkv])
            cmpT = K.sb("cmpT", [128, 4, T], BF16)
            xb = [K.sb("xb", [128, D], F32) for _ in range(2)]
            hb = K.sb("hb1", [128, D], BF16)
            hT4 = K.sb("hT4", [128, 8, 512], BF16)
            stg = [K.sb("stg", [128, 512], BF16) for _ in range(2)]
            vst = [K.sb("vst", [128, 2, 2, 130], BF16) for _ in range(2)]
            for t in vst:
                K.op("dve", lambda e, t=t: e.memset(t[:, :, :, 128:129], 1.0), writes=[t])
                K.op("dve", lambda e, t=t: e.memset(t[:, :, :, 129:130], 0.0), writes=[t])
            fm_cols = [(0, "c", 0), (128, "c", 1), (256, "c", 2), (384, "c", 3), (512, "s", 0), (640, "s", 1), (1024, "w", 0), (1152, "w", 1)]
            cnt_stg = 0
            cnt_v = 0
            hT4s = [hT4, K.sb("hT4b", [128, 8, 512], BF16)]

            def b1_stage1(tg):
                for sub in range(4):
                    i = tg * 4 + sub
                    x = xb[i % 2]
                    K.dma("sp", x[:], xs.h[i * 128:(i + 1) * 128, :], reads=[xs], writes=[x])
                    rmsnorm_rows(K, c, x[:], x, D, gk[:], gk, hb[:], hb, "a")
                    transposes(K, c, [hb[:, k * 128:(k + 1) * 128] for k in range(8)], [hb], c.PM,
                               hT4s[tg % 2][:, :, sub * 128:(sub + 1) * 128], hT4s[tg % 2], 128, 128, copy_eng="act" if sub % 2 == 0 else "dve")

            b1_stage1(0)
            for tg in range(T // 512):
                if tg + 1 < T // 512:
                    b1_stage1(tg + 1)
                hT4 = hT4s[tg % 2]
                for ci, (col0, kind, idx) in enumerate(fm_cols):
                    P = c.PL[ci % 2]
                    for k in range(8):
                        K.op("pe", lambda e, k=k, P=P, col0=col0: e.matmul(P[:, 0:512], lhsT=wkv[:, k, col0:col0 + 128], rhs=hT4[:, k, :],
                                                                           start=(k == 0), stop=(k == 7)), reads=[wkv, hT4], writes=[P])
                    if kind == "c":
                        K.op("act", lambda e, P=P, idx=idx: e.activation(out=cmpT[:, idx, tg * 512:(tg + 1) * 512], in_=P[:, 0:512], func=AF.Copy),
                             reads=[P], writes=[cmpT])
                    else:
                        st = stg[cnt_stg % 2]
                        cnt_stg += 1
                        K.op("dve", lambda e, P=P, st=st: e.tensor_copy(out=st[:], in_=P[:, 0:512]), reads=[P], writes=[st])
                        dst = kslcT_d if kind == "s" else kwinT_d
                        K.dma("pool", dst.h[idx, :, tg * 512:(tg + 1) * 512], st[:], reads=[st], writes=[dst])
                for sub in range(4):
                    i = tg * 4 + sub
                    vt = vst[cnt_v % 2]
                    cnt_v += 1
                    P = c.PL[sub % 2]
                    for br, col0 in enumerate((768, 1280)):
                        for k in range(8):
                            K.op("pe", lambda e, k=k, P=P, col0=col0, br=br: e.matmul(
                                P[:, br * 256:(br + 1) * 256], lhsT=hT4[:, k, sub * 128:(sub + 1) * 128], rhs=wkv[:, k, col0:col0 + 256],
                                start=(k == 0), stop=(k == 7)), reads=[hT4, wkv], writes=[P])
                    K.op("act", lambda e, P=P, vt=vt: e.activation(
                        out=vt[:, :, :, 0:128], in_=P[:, 0:512].rearrange("p (b g d) -> p b g d", b=2, g=2), func=AF.Copy),
                         reads=[P], writes=[vt])
                    K.dma("pool", vslc_d.h[i * 128:(i + 1) * 128, :], vt[:, 0, :, :].rearrange("p g d -> p (g d)"), reads=[vt], writes=[vslc_d])
                    K.dma("pool", vwin_d.h[i * 128:(i + 1) * 128, :], vt[:, 1, :, :].rearrange("p g d -> p (g d)"), reads=[vt], writes=[vwin_d])
            w1 = K.sb("w1", [128, CMP_LEN, CMP_HID], BF16)
            w2 = K.sb("w2", [128, 2, HD], BF16)
            posr = K.sb("posr", [CMP_LEN, HD], BF16)
            posT = K.sb("posT", [128, CMP_LEN], BF16)
            c0 = K.sb("c0", [128, 2], F32)
            hid = K.sb("hid", [128, 2, NCP], BF16)
            K.op("dve", lambda e: e.memset(hid[:], 0.0), writes=[hid])
            u = K.sb("u", [128, NCP], F32)
            t1 = K.sb("t1", [128, NCP], F32)
            kcT = K.sb("kcT", [128, 2, NCP], BF16)
            K.op("dve", lambda e: e.memset(kcT[:], 0.0), writes=[kcT])
            vca = K.sb("vca", [128, NCC, 2, 130], BF16)
            K.op("dve", lambda e: e.memset(vca[:], 0.0), writes=[vca])
            K.op("dve", lambda e: e.memset(vca[:, :, :, 128:129], 1.0), writes=[vca])
            for kv, (w1_bf, w2_bf, pos_bf) in enumerate(((w1k_bf, w2k_bf, posk_bf), (w1v_bf, w2v_bf, posv_bf))):
                K.dma("sp", w1[:], w1_bf.h.rearrange("(l p) n -> p l n", p=128), reads=[w1_bf], writes=[w1])
                K.dma("sp", w2[:], w2_bf.h.rearrange("(k p) n -> p k n", p=128), reads=[w2_bf], writes=[w2])
                K.dma("sp", posr[:], pos_bf.h, reads=[pos_bf], writes=[posr])
                transposes(K, c, [posr[:, :]], [posr], c.PM, posT[:], posT, 128, CMP_LEN)
                for hc in range(2):
                    for l in range(CMP_LEN):
                        K.op("pe", lambda e, l=l, hc=hc: e.matmul(c.PM[:, hc * 2:hc * 2 + 1], lhsT=w1[:, l, hc * 128:(hc + 1) * 128], rhs=posT[:, l:l + 1],
                                                                  start=(l == 0), stop=(l == CMP_LEN - 1)), reads=[w1, posT], writes=[c.PM])
                    K.op("dve", lambda e, hc=hc: e.tensor_copy(out=c0[:, hc:hc + 1], in_=c.PM[:, hc * 2:hc * 2 + 1]), reads=[c.PM], writes=[c0])
                for g in range(2):
                    src = cmpT[:, kv * 2 + g, :]
                    for hc in range(2):
                        P = c.PL[hc]
                        for l in range(CMP_LEN):
                            rhs = bass.AP(tensor=src.tensor, offset=src.offset + l, ap=[list(src.ap[0]), [CMP_STRIDE, n_cmp]])
                            K.op("pe", lambda e, l=l, hc=hc, rhs=rhs, P=P: e.matmul(P[:, 0:n_cmp], lhsT=w1[:, l, hc * 128:(hc + 1) * 128], rhs=rhs,
                                                                                    start=(l == 0), stop=(l == CMP_LEN - 1)), reads=[w1, cmpT], writes=[P])
                        K.op("act", lambda e, hc=hc, P=P: e.activation(out=u[:, 0:n_cmp], in_=P[:, 0:n_cmp], func=AF.Identity, bias=c0[:, hc:hc + 1]),
                             reads=[P, c0], writes=[u])
                        K.op("dve", lambda e: e.tensor_tensor(out=t1[:, 0:n_cmp], in0=u[:, 0:n_cmp], in1=u[:, 0:n_cmp], op=ALU.mult), reads=[u], writes=[t1])
                        K.op("dve", lambda e: e.tensor_scalar(out=t1[:, 0:n_cmp], in0=t1[:, 0:n_cmp], scalar1=0.044715, scalar2=1.0, op0=ALU.mult, op1=ALU.add),
                             reads=[t1], writes=[t1])
                        K.op("dve", lambda e: e.tensor_tensor(out=t1[:, 0:n_cmp], in0=t1[:, 0:n_cmp], in1=u[:, 0:n_cmp], op=ALU.mult), reads=[t1, u], writes=[t1])
                        K.op("act", lambda e: e.activation(out=t1[:, 0:n_cmp], in_=t1[:, 0:n_cmp], func=AF.Tanh, scale=float(math.sqrt(2.0 / math.pi))),
                             reads=[t1], writes=[t1])
                        K.op("dve", lambda e: e.tensor_scalar(out=t1[:, 0:n_cmp], in0=t1[:, 0:n_cmp], scalar1=0.5, scalar2=0.5, op0=ALU.mult, op1=ALU.add),
                             reads=[t1], writes=[t1])
                        K.op("dve", lambda e, hc=hc: e.tensor_tensor(out=hid[:, hc, 0:n_cmp], in0=t1[:, 0:n_cmp], in1=u[:, 0:n_cmp], op=ALU.mult),
                             reads=[t1, u], writes=[hid])
                    if kv == 0:
                        P = c.PL[0]
                        for hc in range(2):
                            K.op("pe", lambda e, hc=hc, P=P: e.matmul(P[:, 0:n_cmp], lhsT=w2[:, hc, :], rhs=hid[:, hc, 0:n_cmp],
                                                                      start=(hc == 0), stop=(hc == 1)), reads=[w2, hid], writes=[P])
                        K.op("act", lambda e, g=g, P=P: e.activation(out=kcT[:, g, 0:n_cmp], in_=P[:, 0:n_cmp], func=AF.Copy), reads=[P], writes=[kcT])
                    else:
                        for cc in range(NCC):
                            P = c.PL[cc % 2]
                            for hc in range(2):
                                K.op("pe", lambda e, hc=hc, P=P, cc=cc: e.matmul(P[:, 0:128], lhsT=hid[:, hc, cc * 128:(cc + 1) * 128], rhs=w2[:, hc, :],
                                                                                 start=(hc == 0), stop=(hc == 1)), reads=[w2, hid], writes=[P])
                            K.op("act", lambda e, g=g, P=P, cc=cc: e.activation(out=vca[:, cc, g, 0:128], in_=P[:, 0:128], func=AF.Copy),
                                 reads=[P], writes=[vca])
            K.dma("pool", kcT_d.h, kcT[:].rearrange("p g n -> p (g n)"), reads=[kcT], writes=[kcT_d])
            K.dma("pool", vca_d.h, vca[:].rearrange("p c g d -> p (c g d)"), reads=[vca], writes=[vca_d])
            K.barrier()
            K.es = saved

        with ExitStack() as es2:
            saved = K.es
            K.es = es2
            ga = K.sb("ga", [128, D], F32)
            load_bcast(K, ga, g_attn, D)
            wbin = K.sb("wbin", [128, 8, 1048], BF16)
            K.dma("sp", wbin[:], win_bf.h.rearrange("(k p) n -> p k n", p=128), reads=[win_bf], writes=[wbin])
            ftab = K.sb("ftab", [128, H, NTAB * 128], BF16)
            K.dma("sp", ftab[:], ftab_bf.h.rearrange("p (h n) -> p h n", h=H), reads=[ftab_bf], writes=[ftab])
            wtab = K.sb("wtab", [128, H, 6 * 128], BF16)
            K.dma("sp", wtab[:], wtab_bf.h.rearrange("p (h n) -> p h n", h=H), reads=[wtab_bf], writes=[wtab])
            kcT = K.sb("kcT", [128, 2, NCP], BF16)
            K.dma("sp", kcT[:], kcT_d.h.rearrange("p (g n) -> p g n", g=2), reads=[kcT_d], writes=[kcT])
            vca = K.sb("vca", [128, NCC, 2, 130], BF16)
            K.dma("sp", vca[:], vca_d.h.rearrange("p (c g d) -> p c g d", c=NCC, g=2), reads=[vca_d], writes=[vca])
            selmap = K.sb("selmap", [128, NCC, NB], BF16)
            K.dma("sp", selmap[:], selmap_bf.h.rearrange("(c p) j -> p c j", p=128), reads=[selmap_bf], writes=[selmap])
            eall = K.sb("eall", [128, T], BF16)
            K.dma("sp", eall[:], eall_bf.h, reads=[eall_bf], writes=[eall])
            xb = [K.sb("xb", [128, D], F32) for _ in range(2)]
            if fused:
                xp2 = [K.sb("xp2", [128, 2, D], F32) for _ in range(2)]
                psel = K.sb("psel", [128, 2], F32)
                K.dma("sp", psel[:], psel_d.h, reads=[psel_d], writes=[psel])
            hb = K.sb("hb2", [128, D], BF16)
            hT = K.sb("hT2", [128, 8, 128], BF16)
            qT = K.sb("qT", [128, H * 128], BF16)
            gates = K.sb("gates", [128, 24], F32)
            scg = K.sb("scg", [128, 8], F32)
            oacc = K.sb("oacc", [128, D], F32)
            oab = K.sb("oab", [128, D], BF16)
            oTs = K.sb("oTs", [128, H, 128], BF16)
            imp = K.sb("imp", [128, 2, NB], F32)
            imt = K.sb("imt", [128, NB], F32)
            m8a = K.sb("m8a", [128, 8], F32)
            m8b = K.sb("m8b", [128, 8], F32)
            selm = K.sb("selm", [128, 2, 128], BF16)
            K.op("dve", lambda e: e.memset(selm[:], 0.0), writes=[selm])
            selT = K.sb("selT", [128, 2, 128], BF16)
            keep = [K.sb("keep", [128, NB], F32) for _ in range(2)]
            addm = [K.sb("addm", [128, NB], F32) for _ in range(2)]
            cm01 = [K.sb("cm01", [128, NCC, 128], BF16) for _ in range(2)]
            PTc = [K.sb("PTc", [128, 512], BF16) for _ in range(2)]
            c.kTc = [K.sb("kTc", [128, 2, 512], BF16) for _ in range(2)]
            c.vc = [K.sb("vc", [128, 4, 260], BF16) for _ in range(2)]
            c.PT = [K.sb("PT", [128, 1024], BF16) for _ in range(2)]
            c.rz = K.sb("rz", [128, 8], F32)

            for j in range(NQ):
                x = xb[j % 2]
                if fused:
                    xp = xp2[j % 2]
                    K.dma("sp", xp[:], xs.h[2 * j * 128:(2 * j + 2) * 128, :].rearrange("(s p) n -> p s n", p=128), reads=[xs], writes=[xp])
                    K.op("dve", lambda e: e.tensor_scalar(out=x[:], in0=xp[:, 0, :], scalar1=psel[:, 0:1], scalar2=None, op0=ALU.mult),
                         reads=[xp, psel], writes=[x])
                    K.op("dve", lambda e: e.scalar_tensor_tensor(out=x[:], in0=xp[:, 1, :], scalar=psel[:, 1:2], in1=x[:], op0=ALU.mult, op1=ALU.add),
                         reads=[xp, psel, x], writes=[x])
                else:
                    K.dma("sp", x[:], xq.h[j * 128:(j + 1) * 128, :], reads=[xq], writes=[x])
                kp, am, cm = keep[j % 2], addm[j % 2], cm01[j % 2]
                K.dma("sp", kp[:], keep_d.h[j * 128:(j + 1) * 128, :], reads=[keep_d], writes=[kp])
                K.dma("sp", am[:], add_d.h[j * 128:(j + 1) * 128, :], reads=[add_d], writes=[am])
                K.dma("sp", cm[:], cm01_bf.h[j * 128:(j + 1) * 128, :].rearrange("p (c q) -> p c q", c=NCC), reads=[cm01_bf], writes=[cm])
                rmsnorm_rows(K, c, x[:], x, D, ga[:], ga, hb[:], hb, "a")
                transposes(K, c, [hb[:, k * 128:(k + 1) * 128] for k in range(8)], [hb], c.PM, hT[:], hT, 128, 128)
                P0 = c.PL[0]
                for h in range(H):
                    for k in range(8):
                        K.op("pe", lambda e, h=h, k=k: e.matmul(P0[:, h * 128:(h + 1) * 128], lhsT=wbin[:, k, h * 128:(h + 1) * 128], rhs=hT[:, k, :],
                                                               start=(k == 0), stop=(k == 7)), reads=[wbin, hT], writes=[P0])
                K.op("dve", lambda e: e.tensor_scalar(out=qT[:], in0=P0[:], scalar1=float(HD ** -0.5), scalar2=None, op0=ALU.mult),
                     reads=[P0], writes=[qT])
                P1 = c.PL[1]
                for k in range(8):
                    K.op("pe", lambda e, k=k: e.matmul(P1[:, 0:24], lhsT=hT[:, k, :], rhs=wbin[:, k, 1024:1048], start=(k == 0), stop=(k == 7)),
                         reads=[hT, wbin], writes=[P1])
                K.op("act", lambda e: e.activation(out=gates[:], in_=P1[:, 0:24], func=AF.Sigmoid), reads=[P1], writes=[gates])

                for g in range(2):
                    for cc in range(NCC):
                        L = c.PL[cc % 2]
                        K.op("pe", lambda e, L=L, cc=cc: e.matmul(L[:, 0:512], lhsT=kcT[:, g, cc * 128:(cc + 1) * 128], rhs=qT[:, g * 512:(g + 1) * 512],
                                                                  start=True, stop=True), reads=[kcT, qT], writes=[L])
                        pt = PTc[cc % 2]
                        K.op("act", lambda e, L=L, pt=pt: e.activation(out=pt[:], in_=L[:, 0:512], func=AF.Exp), reads=[L], writes=[pt])
                        pv = pt[:].rearrange("p (r q) -> p r q", r=4)
                        mv = cm[:, cc, :].rearrange("p (o q) -> p o q", o=1).to_broadcast([128, 4, 128])
                        K.op("dve", lambda e, pv=pv, mv=mv: e.tensor_tensor(out=pv, in0=pv, in1=mv, op=ALU.mult), reads=[pt, cm], writes=[pt])
                        for r in range(4):
                            bank, slot = divmod(r, 3)
                            K.op("pe", lambda e, r=r, bank=bank, slot=slot, pt=pt, cc=cc: e.matmul(
                                c.PO[:, bank, slot * 130:(slot + 1) * 130], lhsT=pt[:, r * 128:(r + 1) * 128], rhs=vca[:, cc, g, :],
                                start=(cc == 0 and slot == 0), stop=(cc == NCC - 1), skip_group_check=True), reads=[pt, vca], writes=[c.PO])
                        for r in range(4):
                            K.op("pe", lambda e, r=r, pt=pt, cc=cc: e.matmul(
                                c.PO[:, 2, r * NB:(r + 1) * NB], lhsT=pt[:, r * 128:(r + 1) * 128], rhs=selmap[:, cc, :],
                                start=(cc == 0 and r == 0), stop=(cc == NCC - 1), skip_group_check=True), reads=[pt, selmap], writes=[c.PO])
                    for bank, n in ((0, 3), (1, 1)):
                        zs = c.PO[:, bank, :]
                        zsrc = bass.AP(tensor=zs.tensor, offset=zs.offset + 128, ap=[list(zs.ap[0]), [130, n], [1, 1]])
                        K.op("dve", lambda e, zsrc=zsrc, bank=bank, n=n: e.tensor_scalar(
                            out=c.rz[:, 3 * bank:3 * bank + n].rearrange("p (n o) -> p n o", o=1), in0=zsrc, scalar1=1e-30, scalar2=None,
                            op0=ALU.max), reads=[c.PO], writes=[c.rz])
                    K.op("dve", lambda e: e.reciprocal(out=c.rz[:, 0:4], in_=c.rz[:, 0:4]), reads=[c.rz], writes=[c.rz])
                    gv = bass.AP(tensor=gates[:].tensor, offset=gates[:].offset + g * 12, ap=[list(gates[:].ap[0]), [3, 4]])
                    K.op("dve", lambda e, gv=gv: e.tensor_tensor(out=scg[:, 0:4], in0=c.rz[:, 0:4], in1=gv, op=ALU.mult),
                         reads=[c.rz, gates], writes=[scg])
                    for r in range(4):
                        bank, slot = divmod(r, 3)
                        h = g * 4 + r
                        K.op("act" if r % 2 == 0 else "dve",
                             (lambda e, h=h, bank=bank, slot=slot, r=r: e.activation(out=oacc[:, h * 128:(h + 1) * 128], in_=c.PO[:, bank, slot * 130:slot * 130 + 128],
                                                                                    func=AF.Copy, scale=scg[:, r:r + 1])) if r % 2 == 0 else
                             (lambda e, h=h, bank=bank, slot=slot, r=r: e.tensor_scalar(out=oacc[:, h * 128:(h + 1) * 128], in0=c.PO[:, bank, slot * 130:slot * 130 + 128],
                                                                                       scalar1=scg[:, r:r + 1], scalar2=None, op0=ALU.mult)),
                             reads=[c.PO, scg], writes=[oacc])
                    K.op("dve", lambda e, g=g: e.tensor_scalar(out=imp[:, g, :], in0=c.PO[:, 2, 0:NB], scalar1=c.rz[:, 0:1], scalar2=None, op0=ALU.mult),
                         reads=[c.PO, c.rz], writes=[imp])
                    for r in range(1, 4):
                        K.op("dve", lambda e, g=g, r=r: e.scalar_tensor_tensor(out=imp[:, g, :], in0=c.PO[:, 2, r * NB:(r + 1) * NB], scalar=c.rz[:, r:r + 1],
                                                                               in1=imp[:, g, :], op0=ALU.mult, op1=ALU.add), reads=[c.PO, c.rz, imp], writes=[imp])
                for g in range(2):
                    K.op("dve", lambda e, g=g: e.tensor_tensor(out=imp[:, g, :], in0=imp[:, g, :], in1=kp[:], op=ALU.mult), reads=[imp, kp], writes=[imp])
                    K.op("dve", lambda e, g=g: e.tensor_tensor(out=imp[:, g, :], in0=imp[:, g, :], in1=am[:], op=ALU.add), reads=[imp, am], writes=[imp])
                    K.op("dve", lambda e, g=g: e.max(out=m8a[:], in_=imp[:, g, :]), reads=[imp], writes=[m8a])
                    K.op("dve", lambda e, g=g: e.match_replace(out=imt[:], in_to_replace=m8a[:], in_values=imp[:, g, :], imm_value=-3.0e38),
                         reads=[imp, m8a], writes=[imt])
                    K.op("dve", lambda e: e.max(out=m8b[:], in_=imt[:]), reads=[imt], writes=[m8b])
                    K.op("dve", lambda e, g=g: e.tensor_scalar(out=selm[:, g, 0:NB], in0=imp[:, g, :], scalar1=m8b[:, 7:8], scalar2=None, op0=ALU.is_ge),
                         reads=[imp, m8b], writes=[selm])
                transposes(K, c, [selm[:, 0, :], selm[:, 1, :]], [selm], c.PM, selT[:], selT, 128, 128, copy_eng="dve")

                def maskfn(k, slot):
                    pm = c.PM[:, slot * 256:(slot + 1) * 256]
                    K.op("pe", lambda e: e.matmul(pm, lhsT=eall[:, k * 128:(k + 1) * 128], rhs=selT[:].rearrange("p g q -> p (g q)"),
                                                  start=True, stop=True), reads=[eall, selT], writes=[c.PM])
                    return pm.rearrange("p (g o q) -> p g o q", g=2, o=1).to_broadcast([128, 2, 4, 128]), c.PM

                def make_put(br, first=False):
                    def gate_fn():
                        gv = bass.AP(tensor=gates[:].tensor, offset=gates[:].offset + br, ap=[list(gates[:].ap[0]), [3, 8]])
                        K.op("dve", lambda e: e.tensor_tensor(out=scg[:], in0=c.rz[:], in1=gv, op=ALU.mult), reads=[c.rz, gates], writes=[scg])

                    def put(h, o_ap, rz_ap):
                        K.op("dve", lambda e: e.scalar_tensor_tensor(out=oacc[:, h * 128:(h + 1) * 128], in0=o_ap, scalar=scg[:, h:h + 1],
                                                                     in1=oacc[:, h * 128:(h + 1) * 128], op0=ALU.mult, op1=ALU.add),
                             reads=[c.PO, scg, oacc], writes=[oacc])
                    return put, gate_fn

                attention(K, c, 2, 4, qT, list(range(2 * j + 2)), kslcT_d, vslc_d, ftab,
                          lambda k: min(2 * j - k + 1, NTAB - 1) * 128, maskfn)
                put, gate_fn = make_put(1)
                attn_norm(K, c, H, put, gate_fn)
                attention(K, c, 2, 4, qT, list(range(max(0, 2 * j - 4), 2 * j + 2)), kwinT_d, vwin_d, wtab,
                          lambda k: (2 * j - k + 1) * 128, None)
                put, gate_fn = make_put(2)
                attn_norm(K, c, H, put, gate_fn)
                K.op("act", lambda e: e.activation(out=oab[:], in_=oacc[:], func=AF.Copy), reads=[oacc], writes=[oab])
                transposes(K, c, [oab[:, h * 128:(h + 1) * 128] for h in range(H)], [oab], c.PM, oTs[:], oTs, 128, 128)
                K.dma("pool", oT_d.h[:, :, j * 128:(j + 1) * 128], oTs[:], reads=[oTs], writes=[oT_d])
            K.barrier()
            K.es = saved

        with ExitStack() as es3:
            saved = K.es
            K.es = es3
            mlp_phase(K, c, xs if fused else xq, oT_d, wo_bf, wup_bf, wdn_bf, g_mlp, out, NQ * 128, final_g=g_fin,
                      blend=psel_d if fused else None)
            if debug:
                for nm, t, shp in (("kslcT", kslcT_d, [256, T]), ("vslc", vslc_d, [T, 260]), ("kcT", kcT_d, [128, 2 * NCP]),
                                   ("vca", vca_d, [128, NCC * 260]), ("oT", oT_d, [128 * H, NQ * 128]), ("kwinT", kwinT_d, [256, T]), ("vwin", vwin_d, [T, 260])):
                    o = K.dram("dbg_" + nm, shp, F32, kind="ExternalOutput")
                    K.dma("pool", o.h, t.h.tensor.reshape(shp).ap(), reads=[t], writes=[o])
            K.barrier()
            K.es = saved


def _rel_bucket(dist):
    dist = jnp.maximum(dist, 0)
    exact = 16
    log_ratio = jnp.log(jnp.maximum(dist, 1).astype(jnp.float32) / exact) / math.log(4096 / exact)
    large = exact + (log_ratio * (32 - exact)).astype(jnp.int32)
    return jnp.where(dist < exact, dist, jnp.minimum(large, 31))


def make_ftab(rel_bias, p, ntab, lo_valid=0, hi_valid=None):
    s = np.arange(128)[:, None]
    col = np.arange(ntab * 128)[None, :]
    dist = col - 128 + 128 * p - s
    with jax.default_device(jax.devices("cpu")[0]):
        b = np.asarray(_rel_bucket(jnp.asarray(dist, dtype=jnp.int32)))
    tab = np.asarray(rel_bias, dtype=np.float32)[b]
    ok = dist >= lo_valid
    if hi_valid is not None:
        ok &= dist < hi_valid
    tab = np.where(ok[:, :, None], tab, np.float32(NEGB))
    return np.ascontiguousarray(np.transpose(tab, (0, 2, 1))).reshape(128, -1).astype(np.float32)


_CACHE = {}


def run_A(T, x, inp, debug=False):
    B = x.shape[0]
    NQ = T // 256
    NTAB = min(26, 2 * NQ)
    if ("A", T, debug) not in _CACHE:
        _CACHE[("A", T, debug)] = build_A(T, debug)
    nc = _CACHE[("A", T, debug)]
    f = lambda a: np.ascontiguousarray(np.asarray(a, dtype=np.float32))
    in_maps = []
    for core in range(N_CORES):
        b, p = core // 2, core % 2
        xb = x[b % B]
        xq = xb.reshape(T // 128, 128, D)[p::2].reshape(NQ * 128, D)
        q = np.arange(128)[:, None]
        cc = np.arange(256)[None, :]
        cmask = np.where(cc > 128 * p + q, np.float32(-1e30), np.float32(0.0)).astype(np.float32)
        in_maps.append({
            "xs": f(xb), "xq": f(xq),
            "a_w_in": f(inp["a_w_in"][0]), "g_attn0": f(inp["g_attn"][0]), "g_mlp0": f(inp["g_mlp"][0]),
            "a_g_q_lat": f(inp["a_g_q_lat"][0]), "a_g_kv_lat": f(inp["a_g_kv_lat"][0]), "a_g_k_idx": f(inp["a_g_k_idx"][0]),
            "a_w_uq": f(inp["a_w_uq"][0]).reshape(QL, H * HD), "a_w_q_idx": f(inp["a_w_q_idx"][0]).reshape(QL, H * IDXD),
            "a_w_uk": f(inp["a_w_uk"][0]).reshape(KVL, H * HD), "a_w_uv": f(inp["a_w_uv"][0]).reshape(KVL, H * HD),
            "a_w_o": f(inp["a_w_o"][0]), "w_up0": f(inp["w_up"][0]), "w_down0": f(inp["w_down"][0]),
            "ftabA": make_ftab(inp["rel_bias"], p, NTAB), "cmask": cmask,
        })
    res = run_bass_kernel_spmd(nc, in_maps, core_ids=list(range(N_CORES)))
    x1 = np.zeros((B, T, D), np.float32)
    for core in range(N_CORES):
        b, p = core // 2, core % 2
        if b < B:
            x1[b].reshape(T // 128, 128, D)[p::2] = res.results[core]["x1"].reshape(NQ, 128, D)
    if debug:
        return x1, res.results
    return x1


def _sel_map(n_cmp, n_slc):
    c0 = CMP_STRIDE * np.arange(n_cmp)[:, None]
    s0 = 64 * np.arange(n_slc)[None, :]
    ov = np.clip(np.minimum(c0 + CMP_LEN, s0 + 64) - np.maximum(c0, s0), 0, None)
    return (ov / CMP_LEN).astype(np.float32)


def b_static_inputs(T, p, inp):
    NQ = T // 256
    NTAB = min(26, 2 * NQ)
    NB = T // 64
    NCP = T // 16
    n_cmp = NCP - 1
    NCC = NCP // 128
    f = lambda a: np.ascontiguousarray(np.asarray(a, dtype=np.float32))
    selmap = np.zeros((NCP, NB), np.float32)
    selmap[:n_cmp] = _sel_map(n_cmp, NB)
    eall = np.zeros((128, T), np.float32)
    eall[np.arange(T) // 64, np.arange(T)] = 1.0
    t = (128 * (2 * np.arange(NQ)[:, None] + p) + np.arange(128)[None, :]).reshape(-1)
    n = np.arange(NCP)
    cm = ((CMP_STRIDE * n[None, :] + CMP_LEN - 1 <= t[:, None]) & (n[None, :] < n_cmp)).astype(np.float32)
    cm01 = cm.reshape(NQ, 128, NCC, 128).transpose(0, 3, 2, 1).reshape(NQ * 128, NCC * 128)
    blk = np.arange(NB)[None, :]
    cur = (t // 64)[:, None]
    forced = (blk == 0) | (blk == cur) | (blk == cur - 1)
    future = blk * 64 > t[:, None]
    keep = (~(forced | future)).astype(np.float32)
    addm = np.where(future, np.float32(-1e30), np.where(forced, np.float32(1e9), np.float32(0.0))).astype(np.float32)
    return {
        "g_kv_shared": f(inp["g_kv_shared"]), "w_kv_shared": f(inp["w_kv_shared"]),
        "cmp_pos_k": f(inp["cmp_pos_k"]), "cmp_pos_v": f(inp["cmp_pos_v"]),
        "cmp_w1_k": f(inp["cmp_w1_k"]), "cmp_w1_v": f(inp["cmp_w1_v"]), "cmp_w2_k": f(inp["cmp_w2_k"]), "cmp_w2_v": f(inp["cmp_w2_v"]),
        "b_w_in": f(inp["b_w_in"][0]), "b_w_o": f(inp["b_w_o"][0]),
        "g_attn1": f(inp["g_attn"][1]), "g_mlp1": f(inp["g_mlp"][1]), "g_final": f(inp["g_final"]),
        "w_up1": f(inp["w_up"][1]), "w_down1": f(inp["w_down"][1]),
        "ftabB": make_ftab(inp["rel_bias"], p, NTAB), "wtab": make_ftab(inp["rel_bias"], p, 6, 0, 512),
        "cm01": f(cm01), "keep": f(keep), "addm": f(addm), "selmap": selmap, "eall": eall,
    }


def a_static_inputs(T, inp):
    f = lambda a: np.ascontiguousarray(np.asarray(a, dtype=np.float32))
    return {
        "a_w_in": f(inp["a_w_in"][0]), "g_attn0": f(inp["g_attn"][0]), "g_mlp0": f(inp["g_mlp"][0]),
        "a_g_q_lat": f(inp["a_g_q_lat"][0]), "a_g_kv_lat": f(inp["a_g_kv_lat"][0]), "a_g_k_idx": f(inp["a_g_k_idx"][0]),
        "a_w_uq": f(inp["a_w_uq"][0]).reshape(QL, H * HD), "a_w_q_idx": f(inp["a_w_q_idx"][0]).reshape(QL, H * IDXD),
        "a_w_uk": f(inp["a_w_uk"][0]).reshape(KVL, H * HD), "a_w_uv": f(inp["a_w_uv"][0]).reshape(KVL, H * HD),
        "a_w_o": f(inp["a_w_o"][0]), "w_up0": f(inp["w_up"][0]), "w_down0": f(inp["w_down"][0]),
    }


def run_B(T, x1, inp, debug=False):
    B = x1.shape[0]
    NQ = T // 256
    if ("B", T, debug) not in _CACHE:
        _CACHE[("B", T, debug)] = build_B(T, debug)
    nc = _CACHE[("B", T, debug)]
    f = lambda a: np.ascontiguousarray(np.asarray(a, dtype=np.float32))
    in_maps = []
    for core in range(N_CORES):
        b, p = core // 2, core % 2
        xb = x1[b % B]
        xq = xb.reshape(T // 128, 128, D)[p::2].reshape(NQ * 128, D)
        m = {"xs": f(xb), "xq": f(xq)}
        m.update(b_static_inputs(T, p, inp))
        in_maps.append(m)
    res = run_bass_kernel_spmd(nc, in_maps, core_ids=list(range(N_CORES)))
    out = np.zeros((B, T, D), np.float32)
    for core in range(N_CORES):
        b, p = core // 2, core % 2
        if b < B:
            out[b].reshape(T // 128, 128, D)[p::2] = res.results[core]["out"].reshape(NQ, 128, D)
    if debug:
        return out, res.results
    return out


def run_F(T, x, inp):
    B = x.shape[0]
    NQ = T // 256
    NS = T // 128
    if ("F", T) not in _CACHE:
        _CACHE[("F", T)] = build_F(T)
    nc = _CACHE[("F", T)]
    f = lambda a: np.ascontiguousarray(np.asarray(a, dtype=np.float32))
    a_st = a_static_inputs(T, inp)
    q = np.arange(128)[:, None]
    cc = np.arange(128)[None, :]
    cmask = np.where(cc > q, np.float32(-1e30), np.float32(0.0)).astype(np.float32)
    ftabA = make_ftab(inp["rel_bias"], 1, min(26, NS))
    in_maps = []
    for core in range(N_CORES):
        b, p = core // 2, core % 2
        m = {"xs": f(x[b % B]), "ftabA": ftabA, "cmask": cmask,
             "psel": np.ascontiguousarray(np.broadcast_to(np.array([1.0 - p, float(p)], np.float32), (128, 2)))}
        m.update(a_st)
        m.update(b_static_inputs(T, p, inp))
        in_maps.append(m)
    res = run_bass_kernel_spmd(nc, in_maps, core_ids=list(range(N_CORES)))
    out = np.zeros((B, T, D), np.float32)
    for core in range(N_CORES):
        b, p = core // 2, core % 2
        if b < B:
            out[b].reshape(T // 128, 128, D)[p::2] = res.results[core]["out"].reshape(NQ, 128, D)
    return out


def kernel(**inputs):
    x = np.asarray(inputs["x"], dtype=np.float32)
    B, T, _ = x.shape
    return run_F(T, x, inputs)
```

```python
import math
from contextlib import ExitStack

import numpy as np
import ml_dtypes
import jax
import jax.numpy as jnp

import concourse.bass as bass
import concourse.mybir as mybir
from concourse.bass_utils import run_bass_kernel_spmd

F32 = mybir.dt.float32
BF16 = mybir.dt.bfloat16
U8 = mybir.dt.uint8
AF = mybir.ActivationFunctionType
ALU = mybir.AluOpType
AX = mybir.AxisListType

D = 1024
H = 8
HD = 128
QL = 256
KVL = 128
IDXD = 64
TOPK = 256
DFF = 4096
EPS = 1e-6
NEGB = -30000.0
NIT = 18
EPOCH = 30000
N_CORES = 8


class Tile:
    def __init__(self, h, name, is_dram=False):
        self.h = h.ap() if is_dram else h
        self.name = name
        self.w = None
        self.r = {}

    def __getitem__(self, idx):
        return self.h[idx]


class KB:
    def __init__(self, nc):
        self.nc = nc
        self.es = ExitStack()
        self.engs = {"pe": nc.tensor, "act": nc.scalar, "dve": nc.vector, "pool": nc.gpsimd, "sp": nc.sync}
        self.cnt = {e: 0 for e in self.engs}
        self.esems = {e: [] for e in self.engs}
        self.waited = {e: {} for e in self.engs}
        self.dma_sems = []
        self.dma_uses = []
        self.dma_rng = {"sp": (0, 28), "pool": (28, 40), "act": (28, 40)}
        self.dma_next = {"sp": 0, "pool": 28, "act": 28}
        self.uid = 0

    def sb(self, name, shape, dtype):
        self.uid += 1
        return Tile(self.es.enter_context(self.nc.sbuf_tensor(f"{name}_{self.uid}", list(shape), dtype)), name)

    def ps(self, name, shape, dtype=F32):
        self.uid += 1
        return Tile(self.es.enter_context(self.nc.psum_tensor(f"{name}_{self.uid}", list(shape), dtype)), name)

    def dram(self, name, shape, dtype, kind="Internal"):
        return Tile(self.nc.dram_tensor(name, list(shape), dtype, kind=kind), name, is_dram=True)

    def view(self, t, ap, name=None):
        n = Tile.__new__(Tile)
        n.h = ap
        n.name = name or t.name
        n.w = None
        n.r = {}
        return n

    def alias(self, t, name=None):
        n = Tile.__new__(Tile)
        n.h = t.h
        n.name = name or t.name
        n.w = None
        n.r = {}
        return n

    def _esem(self, e, epoch):
        lst = self.esems[e]
        while len(lst) <= epoch:
            lst.append(self.es.enter_context(self.nc.semaphore(f"s_{e}_{len(lst)}")))
        return lst[epoch]

    def _dsem(self, i):
        while len(self.dma_sems) <= i:
            self.dma_sems.append(self.es.enter_context(self.nc.semaphore(f"s_dma_{len(self.dma_sems)}")))
            self.dma_uses.append(0)
        return self.dma_sems[i]

    def _emit_wait(self, e, key, val):
        w = self.waited[e]
        if w.get(key, 0) >= val:
            return
        eng = self.engs[e]
        if key[0] == "dma":
            eng.wait_ge(self._dsem(key[1]), val)
        else:
            f = key[1]
            epoch, v = (val - 1) // EPOCH, (val - 1) % EPOCH + 1
            eng.wait_ge(self._esem(f, epoch), v)
        w[key] = val

    def _deps(self, e, reads, writes):
        need = {}

        def add(ev):
            if ev is None:
                return
            k, v = ev
            if need.get(k, 0) < v:
                need[k] = v

        for t in reads:
            add(t.w)
        for t in writes:
            add(t.w)
            for k, v in t.r.items():
                add((k, v))
        for k, v in need.items():
            if k == ("eng", "pe") and e == "pe":
                continue
            self._emit_wait(e, k, v)

    def _record(self, ev, reads, writes):
        k, v = ev
        for t in reads:
            if t.r.get(k, 0) < v:
                t.r[k] = v
        for t in writes:
            t.w = ev
            t.r = {}

    def op(self, e, fn, reads=(), writes=()):
        self._deps(e, reads, writes)
        ins = fn(self.engs[e])
        self.cnt[e] += 1
        seq = self.cnt[e]
        epoch = (seq - 1) // EPOCH
        ins.then_inc(self._esem(e, epoch), 1)
        self._record((("eng", e), seq), reads, writes)
        return ins

    def dma(self, q, out, in_, reads=(), writes=(), **kw):
        self._deps(q, reads, writes)
        lo_, hi_ = self.dma_rng[q]
        i = self.dma_next[q]
        self.dma_next[q] = lo_ + (i + 1 - lo_) % (hi_ - lo_)
        sem = self._dsem(i)
        key = ("dma", i)
        if self.dma_uses[i] > 0:
            self._emit_wait(q, key, 16 * self.dma_uses[i])
        self.dma_uses[i] += 1
        val = 16 * self.dma_uses[i]
        self.engs[q].dma_start(out=out, in_=in_, **kw).then_inc(sem, 16)
        self._record((key, val), reads, writes)

    def barrier(self):
        for e in self.engs:
            for i in range(len(self.dma_sems)):
                if self.dma_uses[i] > 0:
                    self._emit_wait(e, ("dma", i), 16 * self.dma_uses[i])
            for f in ("pe", "act", "dve", "pool"):
                if f != e and self.cnt[f] > 0:
                    self._emit_wait(e, ("eng", f), self.cnt[f])

    def finish(self):
        for i in range(len(self.dma_sems)):
            if self.dma_uses[i] > 0:
                self._emit_wait("sp", ("dma", i), 16 * self.dma_uses[i])
        for e in ("pe", "act", "dve", "pool"):
            if self.cnt[e] > 0:
                self._emit_wait("sp", ("eng", e), self.cnt[e])


def bc_ap(ap, dims):
    return bass.AP(tensor=ap.tensor, offset=ap.offset, ap=[list(ap.ap[0])] + [list(d) for d in dims])


class Ctx:
    pass


def make_ident(K, c):
    c.identf = K.sb("identf", [128, 128], F32)
    c.ident = K.sb("ident", [128, 128], BF16)
    K.op("pool", lambda e: e.iota(c.identf[:], pattern=[[1, 128]], base=0, channel_multiplier=-1,
                                  allow_small_or_imprecise_dtypes=True), writes=[c.identf])
    K.op("dve", lambda e: e.tensor_scalar(out=c.ident[:], in0=c.identf[:], scalar1=0.0, scalar2=None,
                                          op0=ALU.is_equal), reads=[c.identf], writes=[c.ident])


def cast_to_scratch(K, dst, src, nelem):
    C = 1024
    while nelem % C:
        C //= 2
    R = nelem // C
    dflat = dst.h.tensor.reshape([R, C]).ap() if hasattr(dst.h.tensor, "reshape") else None
    sflat = src.h.tensor.reshape([R, C]).ap()
    step = 4096
    for r0 in range(0, R, step):
        r1 = min(R, r0 + step)
        K.dma("pool", dflat[r0:r1, :], sflat[r0:r1, :], reads=[src], writes=[dst])


def load_bcast(K, dst, src_tile, n):
    src_ap = bass.AP(tensor=src_tile.h.tensor, offset=0, ap=[[0, 128], [1, n]])
    K.dma("sp", dst[:], src_ap, reads=[src_tile], writes=[dst])


def rmsnorm_rows(K, c, x_ap, xt, n, g_ap, gt, out_ap, ot, tag):
    ss = c.stat[tag + "_ss"]
    rs = c.stat[tag + "_rs"]
    K.op("act", lambda e: e.activation(out=c.junk_bf[:, 0:n], in_=x_ap, func=AF.Square, accum_out=ss[:]),
         reads=[xt], writes=[c.junk_bf, ss])
    K.op("act", lambda e: e.activation(out=rs[:], in_=ss[:], func=AF.Sqrt, scale=1.0 / n, bias=c.eps_t[:]),
         reads=[ss, c.eps_t], writes=[rs])
    K.op("dve", lambda e: e.reciprocal(out=rs[:], in_=rs[:]), reads=[rs], writes=[rs])
    K.op("dve", lambda e: e.scalar_tensor_tensor(out=out_ap, in0=x_ap, scalar=rs[:], in1=g_ap, op0=ALU.mult,
                                                 op1=ALU.mult), reads=[xt, rs, gt], writes=[ot])


def transposes(K, c, src_aps, src_tiles, pt, dst_ap, dst_tile, nparts_out, ncols_each, copy_eng="act"):
    pv = pt[:].bitcast(BF16)
    off = 0
    n = len(src_aps)
    for i, sap in enumerate(src_aps):
        rows = sap.shape[0]
        o = pv[0:nparts_out, off:off + rows]
        K.op("pe", lambda e, o=o, sap=sap, rows=rows: e.transpose(out=o, in_=sap, identity=c.ident[0:rows, 0:rows]),
             reads=list(src_tiles) + [c.ident], writes=[pt])
        off += rows
    srcv = pv[0:nparts_out, 0:off]
    if copy_eng == "act":
        K.op("act", lambda e: e.activation(out=dst_ap, in_=srcv, func=AF.Copy), reads=[pt], writes=[dst_tile])
    else:
        K.op("dve", lambda e: e.tensor_copy(out=dst_ap, in_=srcv), reads=[pt], writes=[dst_tile])


def attention(K, c, G, R, qT, sblocks, kT_dram, v_dram, tab, tab_of, maskfn, CH=4):
    nb = len(sblocks)
    chunks = [sblocks[i:i + CH] for i in range(0, nb, CH)]
    state = {"ci": -1}

    def load_chunk(ci):
        ch = chunks[ci]
        s0 = ch[0] * 128
        n = len(ch)
        kt = c.kTc[ci % 2]
        vt = c.vc[ci % 2]
        ksrc = bass.AP(tensor=kT_dram.h.tensor, offset=s0, ap=[[c.T, 128], [128 * c.T, G], [1, n * 128]])
        K.dma("sp", kt[:, :, 0:n * 128], ksrc, reads=[kT_dram], writes=[kt])
        vsrc = bass.AP(tensor=v_dram.h.tensor, offset=s0 * G * 130, ap=[[G * 130, 128], [128 * G * 130, n], [1, G * 130]])
        K.dma("sp", vt[:, 0:n, :], vsrc, reads=[v_dram], writes=[vt])

    def emit_qk(i):
        k = sblocks[i]
        ci, wi = divmod(i, CH)
        kt = c.kTc[ci % 2]
        L = c.PL[i % 2]
        to = tab_of(k)
        GH = G * R // 2
        for half in range(2):
            g = (half * GH) // R
            K.op("pe", lambda e, half=half, g=g: e.matmul(
                L[:, half * 512:(half + 1) * 512],
                lhsT=kt[:, g, wi * 128:(wi + 1) * 128],
                rhs=qT[:, half * 512:(half + 1) * 512], start=True, stop=False),
                 reads=[kt, qT], writes=[L])
            K.op("pe", lambda e, half=half: e.matmul(
                L[:, half * 512:(half + 1) * 512],
                lhsT=c.ident[:],
                rhs=tab[:, half * GH:(half + 1) * GH, to:to + 128], start=False, stop=True),
                 reads=[c.ident, tab], writes=[L])
        if maskfn is not None:
            state[("m", i)] = maskfn(k, i % 2)

    load_chunk(0)
    emit_qk(0)
    for i in range(nb):
        k = sblocks[i]
        ci, wi = divmod(i, CH)
        if wi == 0 and ci + 1 < len(chunks):
            load_chunk(ci + 1)
        if i + 1 < nb:
            emit_qk(i + 1)
        L = c.PL[i % 2]
        PT = c.PT[i % 2]
        K.op("act", lambda e: e.activation(out=PT[:], in_=L[:], func=AF.Exp), reads=[L], writes=[PT])
        if maskfn is not None:
            map_, mt = state.pop(("m", i))
            pv = PT[:].rearrange("p (g r q) -> p g r q", g=G, r=R)
            K.op("dve", lambda e: e.tensor_tensor(out=pv, in0=pv, in1=map_, op=ALU.mult), reads=[PT, mt], writes=[PT])
        vt = c.vc[ci % 2]
        for h in range(G * R):
            g = h // R
            bank, slot = divmod(h, 3)
            K.op("pe", lambda e, h=h, g=g, bank=bank, slot=slot: e.matmul(
                c.PO[:, bank, slot * 130:(slot + 1) * 130],
                lhsT=PT[:, h * 128:(h + 1) * 128],
                rhs=vt[:, wi, g * 130:(g + 1) * 130],
                start=(i == 0 and slot == 0), stop=(i == nb - 1), skip_group_check=True),
                 reads=[PT, vt], writes=[c.PO])


def attention_h(K, c, G, R, qT, sblocks, kT_dram, v_dram, tab, tab_of, maskfn, CH=4, mult_eng="dve", mask_dram=None):
    nb = len(sblocks)
    chunks = [sblocks[i:i + CH] for i in range(0, nb, CH)]
    masks = {}
    GH = G * R // 2

    def load_chunk(ci):
        ch = chunks[ci]
        s0 = ch[0] * 128
        n = len(ch)
        kt = c.kTc[ci % 2]
        vt = c.vc[ci % 2]
        ksrc = bass.AP(tensor=kT_dram.h.tensor, offset=s0, ap=[[c.T, 128], [128 * c.T, G], [1, n * 128]])
        K.dma("sp", kt[:, :, 0:n * 128], ksrc, reads=[kT_dram], writes=[kt])
        vsrc = bass.AP(tensor=v_dram.h.tensor, offset=s0 * G * 130, ap=[[G * 130, 128], [128 * G * 130, n], [1, G * 130]])
        K.dma("sp", vt[:, 0:n, :], vsrc, reads=[v_dram], writes=[vt])
        if mask_dram is not None:
            mt_ = c.mch[ci % 2]
            K.dma("sp", mt_[:, 0:n * 128], mask_dram.h[:, s0:s0 + n * 128], reads=[mask_dram], writes=[mt_])

    def emit_qk(i):
        k = sblocks[i]
        ci, wi = divmod(i, CH)
        kt = c.kTc[ci % 2]
        to = tab_of(k)
        for half in range(2):
            L = c.Lh[half]
            g = (half * GH) // R
            K.op("pe", lambda e, half=half, g=g, L=L: e.matmul(
                L[:, 0:512], lhsT=kt[:, g, wi * 128:(wi + 1) * 128],
                rhs=qT[:, half * 512:(half + 1) * 512], start=True, stop=False), reads=[kt, qT], writes=[L])
            K.op("pe", lambda e, half=half, L=L: e.matmul(
                L[:, 0:512], lhsT=c.ident[:],
                rhs=tab[:, half * GH:(half + 1) * GH, to:to + 128], start=False, stop=True), reads=[c.ident, tab], writes=[L])
        if maskfn is not None:
            if mask_dram is not None:
                masks[i] = maskfn(k, i % 2, c.mch[ci % 2], wi)
            else:
                masks[i] = maskfn(k, i % 2)

    load_chunk(0)
    emit_qk(0)
    for i in range(nb):
        ci, wi = divmod(i, CH)
        if wi == 0 and ci + 1 < len(chunks):
            load_chunk(ci + 1)
        PT = c.PT[i % 2]
        if ("pth", i % 2) not in c.__dict__:
            c.__dict__[("pth", i % 2)] = [K.view(PT, PT[:, 0:512], "PTa"), K.view(PT, PT[:, 512:1024], "PTb")]
        PTh = c.__dict__[("pth", i % 2)]
        for half in range(2):
            L = c.Lh[half]
            K.op("act", lambda e, half=half, L=L: e.activation(out=PTh[half][:, 0:512], in_=L[:, 0:512], func=AF.Exp),
                 reads=[L], writes=[PTh[half]])
        if i + 1 < nb:
            emit_qk(i + 1)
        if maskfn is not None:
            map_, mt = masks.pop(i)
            for half in range(2):
                pv = PTh[half][:, 0:512].rearrange("p (r q) -> p r q", r=GH)
                if G == 1:
                    mv = mt[:].rearrange("p (o q) -> p o q", o=1).to_broadcast([128, GH, 128])
                else:
                    mv = map_[:, half, :, :]
                K.op(mult_eng, lambda e, pv=pv, mv=mv: e.tensor_tensor(out=pv, in0=pv, in1=mv, op=ALU.mult),
                     reads=[PTh[half], mt], writes=[PTh[half]])
        vt = c.vc[ci % 2]
        for h in range(G * R):
            g = h // R
            bank, slot = divmod(h, 3)
            half, hh = divmod(h, GH)
            K.op("pe", lambda e, h=h, g=g, bank=bank, slot=slot, half=half, hh=hh: e.matmul(
                c.PO[:, bank, slot * 130:(slot + 1) * 130],
                lhsT=PTh[half][:, hh * 128:(hh + 1) * 128],
                rhs=vt[:, wi, g * 130:(g + 1) * 130],
                start=(i == 0 and slot == 0), stop=(i == nb - 1), skip_group_check=True),
                 reads=[PTh[half], vt], writes=[c.PO])
        if i % 2 == 1 or i == nb - 1:
            yield 4.8


def attn_norm(K, c, nheads, out_fn, gate_fn=None):
    for bank in range(3):
        n = min(3, nheads - 3 * bank)
        if n <= 0:
            break
        zsrc = bc_ap(c.PO[:, bank, :], [[130, n], [1, 1]])
        zsrc = bass.AP(tensor=zsrc.tensor, offset=zsrc.offset + 128, ap=zsrc.ap)
        K.op("dve", lambda e, zsrc=zsrc, bank=bank, n=n: e.tensor_scalar(
            out=c.rz[:, 3 * bank:3 * bank + n].rearrange("p (n o) -> p n o", o=1), in0=zsrc, scalar1=1e-30, scalar2=None,
            op0=ALU.max), reads=[c.PO], writes=[c.rz])
    K.op("dve", lambda e: e.reciprocal(out=c.rz[:, 0:nheads], in_=c.rz[:, 0:nheads]), reads=[c.rz], writes=[c.rz])
    if gate_fn is not None:
        gate_fn()
    for h in range(nheads):
        bank, slot = divmod(h, 3)
        out_fn(h, c.PO[:, bank, slot * 130:slot * 130 + 128], c.rz[:, h:h + 1])


def mlp_phase(K, c, src_x, oT_dram, wo_bf, wup_bf, wdn_bf, g_mlp, out_dram, ntok, final_g=None, blend=None):
    TT = 256
    NSUB = TT // 128
    wo = K.sb("wo", [128, H, D], BF16)
    K.dma("sp", wo[:], wo_bf.h.rearrange("(h p) n -> p h n", p=128), reads=[wo_bf], writes=[wo])
    wdn = K.sb("wdn", [128, DFF // 128, D], BF16)
    for q in range(4):
        K.dma("sp", wdn[:, q * 8:(q + 1) * 8, :], wdn_bf.h[q * 1024:(q + 1) * 1024, :].rearrange("(c p) n -> p c n", p=128),
              reads=[wdn_bf], writes=[wdn])
    gm = K.sb("gm", [128, D], F32)
    load_bcast(K, gm, g_mlp, D)
    gf = None
    if final_g is not None:
        gf = K.sb("gf", [128, D], F32)
        load_bcast(K, gf, final_g, D)
    wup = [K.sb("wup", [128, 8, 512], BF16) for _ in range(2)]
    xt = [K.sb("xt", [128, NSUB, D], F32) for _ in range(2)]
    if blend is not None:
        xpp = [K.sb("xpp", [128, 2 * NSUB, D], F32) for _ in range(2)]
        psel = K.sb("pselm", [128, 2], F32)
        K.dma("sp", psel[:], blend.h, reads=[blend], writes=[psel])
    oT = [K.sb("oT", [128, H, TT], BF16) for _ in range(2)]
    hb = K.sb("hb", [128, NSUB, D], BF16)
    hT = K.sb("hT", [128, 8, TT], BF16)
    uT = K.sb("uT", [128, DFF // 128, TT], BF16)
    tmp = [K.sb("tmp", [128, TT], F32) for _ in range(2)]
    yo = [K.sb("yo", [128, D], F32) for _ in range(2)]
    wup_ctr = [0]

    def load_wup(fg):
        t = wup[wup_ctr[0] % 2]
        wup_ctr[0] += 1
        K.dma("sp", t[:], wup_bf.h[:, fg * 512:(fg + 1) * 512].rearrange("(k p) n -> p k n", p=128),
              reads=[wup_bf], writes=[t])
        return t

    ntiles = ntok // TT
    for ti in range(ntiles):
        x = xt[ti % 2]
        o = oT[ti % 2]
        if blend is None:
            K.dma("sp", x[:], src_x.h[ti * TT:(ti + 1) * TT, :].rearrange("(s p) n -> p s n", p=128), reads=[src_x], writes=[x])
        else:
            xp = xpp[ti % 2]
            K.dma("sp", xp[:], src_x.h[2 * ti * TT:2 * (ti + 1) * TT, :].rearrange("(s p) n -> p s n", p=128), reads=[src_x], writes=[xp])
            for s_ in range(NSUB):
                K.op("dve", lambda e, s_=s_: e.tensor_scalar(out=x[:, s_, :], in0=xp[:, 2 * s_, :], scalar1=psel[:, 0:1], scalar2=None, op0=ALU.mult),
                     reads=[xp, psel], writes=[x])
                K.op("dve", lambda e, s_=s_: e.scalar_tensor_tensor(out=x[:, s_, :], in0=xp[:, 2 * s_ + 1, :], scalar=psel[:, 1:2], in1=x[:, s_, :],
                                                                    op0=ALU.mult, op1=ALU.add), reads=[xp, psel, x], writes=[x])
        K.dma("sp", o[:], oT_dram.h[:, :, ti * TT:(ti + 1) * TT], reads=[oT_dram], writes=[o])
        for s in range(NSUB):
            P = c.PL[s % 2]
            for half in range(2):
                for h in range(H):
                    K.op("pe", lambda e, h=h, half=half, s=s, P=P: e.matmul(
                        P[:, half * 512:(half + 1) * 512], lhsT=o[:, h, s * 128:(s + 1) * 128],
                        rhs=wo[:, h, half * 512:(half + 1) * 512], start=(h == 0), stop=(h == H - 1)),
                         reads=[o, wo], writes=[P])
            K.op("dve", lambda e, s=s, P=P: e.tensor_tensor(out=x[:, s, :], in0=x[:, s, :], in1=P[:], op=ALU.add),
                 reads=[x, P], writes=[x])
            rmsnorm_rows(K, c, x[:, s, :], x, D, gm[:], gm, hb[:, s, :], hb, "m")
        for s in range(NSUB):
            transposes(K, c, [hb[:, s, k * 128:(k + 1) * 128] for k in range(8)], [hb], c.PM,
                       hT[:, :, s * 128:(s + 1) * 128], hT, 128, 128,
                       copy_eng="act" if s % 2 == 0 else "dve")
        nxt = load_wup(0)
        for fg in range(8):
            wt = nxt
            if fg + 1 < 8:
                nxt = load_wup(fg + 1)
            for fc in range(4):
                f = fg * 4 + fc
                P = c.PL[f % 2]
                for k in range(8):
                    K.op("pe", lambda e, k=k, fc=fc, P=P, wt=wt: e.matmul(
                        P[:, 0:TT], lhsT=wt[:, k, fc * 128:(fc + 1) * 128], rhs=hT[:, k, :],
                        start=(k == 0), stop=(k == 7)), reads=[wt, hT], writes=[P])
                tm = tmp[f % 2]
                K.op("act", lambda e, P=P, tm=tm: e.activation(out=tm[:], in_=P[:, 0:TT], func=AF.Relu),
                     reads=[P], writes=[tm])
                K.op("dve", lambda e, f=f, tm=tm: e.tensor_tensor(out=uT[:, f, :], in0=tm[:], in1=tm[:], op=ALU.mult),
                     reads=[tm], writes=[uT])
        for s in range(NSUB):
            y = yo[s % 2]
            for half in range(2):
                P = c.PL[half]
                for f in range(DFF // 128):
                    K.op("pe", lambda e, f=f, half=half, s=s, P=P: e.matmul(
                        P[:, 0:512], lhsT=uT[:, f, s * 128:(s + 1) * 128], rhs=wdn[:, f, half * 512:(half + 1) * 512],
                        start=(f == 0), stop=(f == DFF // 128 - 1)), reads=[uT, wdn], writes=[P])
                K.op("dve", lambda e, half=half, s=s, P=P, y=y: e.tensor_tensor(
                    out=y[:, half * 512:(half + 1) * 512], in0=x[:, s, half * 512:(half + 1) * 512], in1=P[:, 0:512],
                    op=ALU.add), reads=[x, P], writes=[y])
            if gf is not None:
                rmsnorm_rows(K, c, y[:], y, D, gf[:], gf, y[:], y, "m")
            r0 = ti * TT + s * 128
            K.dma("pool", out_dram.h[r0:r0 + 128, :], y[:], reads=[y], writes=[out_dram])


def common_setup(K, c, T, NQ):
    c.T = T
    c.NQ = NQ
    c.NS = T // 128
    make_ident(K, c)
    c.eps_t = K.sb("eps", [128, 1], F32)
    K.op("dve", lambda e: e.memset(c.eps_t[:], EPS), writes=[c.eps_t])
    c.junk_bf = K.sb("junk_bf", [128, D], BF16)
    c.stat = {}
    for tag in ("a", "b", "m", "n"):
        c.stat[tag + "_ss"] = K.sb("ss", [128, 1], F32)
        c.stat[tag + "_rs"] = K.sb("rs", [128, 1], F32)
    c.PL = [K.ps("PL0", [128, 1024]), K.ps("PL1", [128, 1024])]
    c.PO = K.ps("PO", [128, 3, 512])
    c.PM = K.ps("PM", [128, 512])


def build_A(T, debug=False):
    nc = bass.Bass("TRN2", target_bir_lowering=False)
    K = KB(nc)
    c = Ctx()
    with K.es:
        common_setup(K, c, T, T // 256)
        emit_A(K, c, T, "par", debug)
        K.finish()
    return nc


def emit_A(K, c, T, mode, debug=False):
    allm = mode == "all"
    NQ = T // 128 if allm else T // 256
    NS = T // 128
    NTAB = min(26, NS if allm else 2 * NQ)
    CW = 128 if allm else 256
    c.NTAB = NTAB
    ein = lambda n, s: K.dram(n, s, F32, kind="ExternalInput")
    xs = ein("xs", [T, D])
    xq = xs if allm else ein("xq", [NQ * 128, D])
    w_in = ein("a_w_in", [D, 456])
    g_attn = ein("g_attn0", [D])
    g_mlp = ein("g_mlp0", [D])
    g_q = ein("a_g_q_lat", [QL])
    g_kv = ein("a_g_kv_lat", [KVL])
    g_ki = ein("a_g_k_idx", [IDXD])
    w_uq = ein("a_w_uq", [QL, H * HD])
    w_qi = ein("a_w_q_idx", [QL, H * IDXD])
    w_uk = ein("a_w_uk", [KVL, H * HD])
    w_uv = ein("a_w_uv", [KVL, H * HD])
    w_o = ein("a_w_o", [D, D])
    w_up = ein("w_up0", [D, DFF])
    w_dn = ein("w_down0", [DFF, D])
    ftab_f = ein("ftabA", [128, H * NTAB * 128])
    cmask_d = ein("cmask", [128, CW])
    if allm:
        x1 = K.dram("x1_all", [T, D], F32)
    else:
        x1 = K.dram("x1", [NQ * 128, D], F32, kind="ExternalOutput")

    sc = lambda n, s, dt=BF16: K.dram("A_" + n, s, dt, kind="Internal")
    win_bf = sc("win_bf", [D, 456])
    wuq_bf = sc("wuq_bf", [QL, H * HD])
    wqi_bf = sc("wqi_bf", [QL, H * IDXD])
    wuk_bf = sc("wuk_bf", [KVL, H * HD])
    wuv_bf = sc("wuv_bf", [KVL, H * HD])
    wo_bf = sc("wo_bf", [D, D])
    wup_bf = sc("wup_bf", [D, DFF])
    wdn_bf = sc("wdn_bf", [DFF, D])
    ftab_bf = sc("ftab_bf", [128, H * NTAB * 128])
    ckvT_d = sc("ckvT_d", [1, 128, T])
    ckva_d = sc("ckva_d", [T, 130])
    kidxT_d = sc("kidxT_d", [IDXD, T])
    oT_d = sc("oT_d", [128, H, NQ * 128])

    if True:
        for dst, src, n in ((win_bf, w_in, D * 456), (wuq_bf, w_uq, QL * H * HD), (wqi_bf, w_qi, QL * H * IDXD),
                            (wuk_bf, w_uk, KVL * H * HD), (wuv_bf, w_uv, KVL * H * HD), (ftab_bf, ftab_f, 128 * H * NTAB * 128),
                            (wo_bf, w_o, D * D), (wup_bf, w_up, D * DFF), (wdn_bf, w_dn, DFF * D)):
            cast_to_scratch(K, dst, src, n)

        with ExitStack() as es1:
            saved = K.es
            K.es = es1
            ga = K.sb("ga", [128, D], F32)
            load_bcast(K, ga, g_attn, D)
            gkv = K.sb("gkv", [128, KVL], F32)
            load_bcast(K, gkv, g_kv, KVL)
            gki = K.sb("gki", [128, IDXD], F32)
            load_bcast(K, gki, g_ki, IDXD)
            wk = K.sb("wk", [128, 8, 192], BF16)
            K.dma("sp", wk[:], win_bf.h[:, 256:448].rearrange("(k p) n -> p k n", p=128), reads=[win_bf], writes=[wk])
            xb = [K.sb("xb", [128, D], F32) for _ in range(2)]
            hb = K.sb("hb1", [128, D], BF16)
            hT = K.sb("hT1", [128, 8, 128], BF16)
            ckv = [K.sb("ckv", [128, 130], BF16) for _ in range(2)]
            kix = [K.sb("kix", [128, IDXD], BF16) for _ in range(2)]
            cT = [K.sb("cT", [128, 128], BF16) for _ in range(2)]
            kT = [K.sb("kT", [IDXD, 128], BF16) for _ in range(2)]
            for t in ckv:
                K.op("dve", lambda e, t=t: e.memset(t[:, 128:129], 1.0), writes=[t])
                K.op("dve", lambda e, t=t: e.memset(t[:, 129:130], 0.0), writes=[t])
            hbs = [hb, K.sb("hb1b", [128, D], BF16)]
            hTs = [hT, K.sb("hT1b", [128, 8, 128], BF16)]

            def a1_stage1(i):
                x = xb[i % 2]
                K.dma("sp", x[:], xs.h[i * 128:(i + 1) * 128, :], reads=[xs], writes=[x])
                rmsnorm_rows(K, c, x[:], x, D, ga[:], ga, hbs[i % 2][:], hbs[i % 2], "n")
                transposes(K, c, [hbs[i % 2][:, k * 128:(k + 1) * 128] for k in range(8)], [hbs[i % 2]], c.PM, hTs[i % 2][:], hTs[i % 2], 128, 128)

            a1_stage1(0)
            for i in range(NS):
                if i + 1 < NS:
                    a1_stage1(i + 1)
                hT = hTs[i % 2]
                P = c.PL[i % 2]
                for k in range(8):
                    K.op("pe", lambda e, k=k, P=P: e.matmul(P[:, 0:192], lhsT=hT[:, k, :], rhs=wk[:, k, :],
                                                           start=(k == 0), stop=(k == 7)), reads=[hT, wk], writes=[P])
                cv = ckv[i % 2]
                ki = kix[i % 2]
                rmsnorm_rows(K, c, P[:, 0:128], P, KVL, gkv[:], gkv, cv[:, 0:128], cv, "a")
                rmsnorm_rows(K, c, P[:, 128:192], P, IDXD, gki[:], gki, ki[:], ki, "b")
                ct = cT[i % 2]
                kt = kT[i % 2]
                transposes(K, c, [cv[:, 0:128]], [cv], c.PM, ct[:], ct, 128, 128, copy_eng="dve")
                transposes(K, c, [ki[:, :]], [ki], c.PM, kt[:], kt, IDXD, 128, copy_eng="act")
                K.dma("pool", ckva_d.h[i * 128:(i + 1) * 128, :], cv[:], reads=[cv], writes=[ckva_d])
                K.dma("pool", ckvT_d.h[0, :, i * 128:(i + 1) * 128], ct[:], reads=[ct], writes=[ckvT_d])
                K.dma("pool", kidxT_d.h[:, i * 128:(i + 1) * 128], kt[:], reads=[kt], writes=[kidxT_d])
            K.barrier()
            K.es = saved

        with ExitStack() as es2:
            saved = K.es
            K.es = es2
            ga = K.sb("ga", [128, D], F32)
            load_bcast(K, ga, g_attn, D)
            gq = K.sb("gq", [128, QL], F32)
            load_bcast(K, gq, g_q, QL)
            wq = K.sb("wq", [128, 8, 264], BF16)
            K.dma("sp", wq[:, :, 0:256], win_bf.h[:, 0:256].rearrange("(k p) n -> p k n", p=128), reads=[win_bf], writes=[wq])
            K.dma("sp", wq[:, :, 256:264], win_bf.h[:, 448:456].rearrange("(k p) n -> p k n", p=128), reads=[win_bf], writes=[wq])
            wuq = K.sb("wuq", [128, 2, H * HD], BF16)
            K.dma("sp", wuq[:], wuq_bf.h.rearrange("(k p) n -> p k n", p=128), reads=[wuq_bf], writes=[wuq])
            wqi = K.sb("wqi", [128, 2, H * IDXD], BF16)
            K.dma("sp", wqi[:], wqi_bf.h.rearrange("(k p) n -> p k n", p=128), reads=[wqi_bf], writes=[wqi])
            wuk = K.sb("wuk", [128, H * HD], BF16)
            K.dma("sp", wuk[:], wuk_bf.h, reads=[wuk_bf], writes=[wuk])
            wuv = K.sb("wuv", [128, H * HD], BF16)
            K.dma("sp", wuv[:], wuv_bf.h, reads=[wuv_bf], writes=[wuv])
            wukT = K.sb("wukT", [128, H, 128], BF16)
            transposes(K, c, [wuk[:, h * 128:(h + 1) * 128] for h in range(H)], [wuk], c.PM, wukT[:], wukT, 128, 128)
            ftab = K.sb("ftab", [128, H, NTAB * 128], BF16)
            K.dma("sp", ftab[:], ftab_bf.h.rearrange("p (h n) -> p h n", h=H), reads=[ftab_bf], writes=[ftab])
            cmask = K.sb("cmask", [128, CW], F32)
            K.dma("sp", cmask[:], cmask_d.h, reads=[cmask_d], writes=[cmask])
            scores = [K.sb("score", [128, T], F32) for _ in range(2)]
            mstage = K.sb("mstage", [128, T], BF16)
            maskD = [K.dram("A_maskD%d" % i, [128, T], BF16) for i in range(2)]
            c.mch = [K.sb("mch", [128, 512], BF16) for _ in range(2)]
            xb = [K.sb("xb", [128, D], F32) for _ in range(1)]
            hb = K.sb("hb2", [128, D], BF16)
            hT = K.sb("hT2", [128, 8, 128], BF16)
            cq = K.sb("cq", [128, QL], BF16)
            cqT = K.sb("cqT", [128, 2, 128], BF16)
            wi = K.sb("wi", [128, 8], F32)
            qTs = K.sb("qTs", [128, H, 128], BF16)
            qaTs = [K.sb("qaT", [128, H * 128], BF16) for _ in range(3)]
            qiT = K.sb("qiT", [IDXD, 128, H], BF16)
            Amat = K.sb("Amat", [128, 128, H], BF16)
            Wg = K.sb("Wg", [128, 8, 128], BF16)
            Rsb = [K.sb("Rsb", [128, 512], BF16) for _ in range(2)]
            kidc = [K.sb("kidc", [IDXD, 512], BF16) for _ in range(2)]
            c.kTc = [K.sb("kTc", [128, 1, 512], BF16) for _ in range(2)]
            c.vc = [K.sb("vc", [128, 4, 130], BF16) for _ in range(2)]
            c.PT = [K.sb("PT", [128, 1024], BF16) for _ in range(2)]
            c.rz = K.sb("rz", [128, 8], F32)
            lo = K.sb("lo", [128, 1], F32)
            hi = K.sb("hi", [128, 1], F32)
            mid = K.sb("mid", [128, 1], F32)
            cnt = K.sb("cnt", [128, 1], F32)
            tsel = K.sb("tsel", [128, 1], F32)
            thrs = [K.sb("thr", [128, 1], F32) for _ in range(2)]
            wtab = K.sb("wtab", [128, NIT + 1], F32)
            p2 = K.sb("p2", [128, NIT + 1], F32)
            for k in range(NIT + 1):
                K.op("dve", lambda e, k=k: e.memset(p2[:, k:k + 1], 2.0 ** (-(k + 1))), writes=[p2])
            mk = [K.sb("mk", [128, 128], BF16) for _ in range(2)]
            mkT = [K.sb("mkT", [128, 128], BF16) for _ in range(2)]
            olat = K.sb("olat", [128, H, 128], BF16)
            olatT = K.sb("olatT", [128, H, 128], BF16)
            oTs = K.sb("oTs", [128, H, 128], BF16)
            c.Lh = [K.view(c.PL[0], c.PL[0][:, 0:512], "L0"), K.view(c.PL[0], c.PL[0][:, 512:1024], "L1")]
            PF = K.view(c.PL[1], c.PL[1][:, 0:512], "PF")
            PSC = K.view(c.PL[1], c.PL[1][:, 512:1024], "PSC")

            def nsb_of(j):
                return (j + 1) if allm else (2 * j + 2)

            def f1(j):
                score, qaT = scores[j % 2], qaTs[j % 3]
                x = xb[0]
                K.dma("sp", x[:], xq.h[j * 128:(j + 1) * 128, :], reads=[xq], writes=[x])
                rmsnorm_rows(K, c, x[:], x, D, ga[:], ga, hb[:], hb, "a")
                transposes(K, c, [hb[:, k * 128:(k + 1) * 128] for k in range(8)], [hb], PF, hT[:], hT, 128, 128)
                yield 3.0
                for k in range(8):
                    K.op("pe", lambda e, k=k: e.matmul(PSC[:, 0:264], lhsT=hT[:, k, :], rhs=wq[:, k, :],
                                                      start=(k == 0), stop=(k == 7)), reads=[hT, wq], writes=[PSC])
                rmsnorm_rows(K, c, PSC[:, 0:256], PSC, QL, gq[:], gq, cq[:], cq, "b")
                K.op("act", lambda e: e.activation(out=wi[:], in_=PSC[:, 256:264], func=AF.Copy,
                                                   scale=float(8 ** -0.5 * IDXD ** -0.5)), reads=[PSC], writes=[wi])
                transposes(K, c, [cq[:, 0:128], cq[:, 128:256]], [cq], PF, cqT[:], cqT, 128, 128)
                yield 3.0
                for hf in range(2):
                    P = PF if hf == 0 else PSC
                    for hh in range(4):
                        h = hf * 4 + hh
                        for rc in range(2):
                            K.op("pe", lambda e, h=h, hh=hh, rc=rc, P=P: e.matmul(P[:, hh * 128:(hh + 1) * 128],
                                                                                lhsT=wuq[:, rc, h * 128:(h + 1) * 128], rhs=cqT[:, rc, :],
                                                                                start=(rc == 0), stop=(rc == 1)), reads=[wuq, cqT], writes=[P])
                    K.op("act", lambda e, hf=hf, P=P: e.activation(out=qTs[:, hf * 4:(hf + 1) * 4, :], in_=P[:, 0:512], func=AF.Copy),
                         reads=[P], writes=[qTs])
                yield 2.0
                for hf in range(2):
                    P = PF if hf == 0 else PSC
                    for hh in range(4):
                        h = hf * 4 + hh
                        K.op("pe", lambda e, h=h, hh=hh, P=P: e.matmul(P[:, hh * 128:(hh + 1) * 128], lhsT=wukT[:, h, :], rhs=qTs[:, h, :],
                                                                     start=True, stop=True), reads=[wukT, qTs], writes=[P])
                    K.op("dve", lambda e, hf=hf, P=P: e.tensor_scalar(out=qaT[:, hf * 512:(hf + 1) * 512], in0=P[:, 0:512], scalar1=float(HD ** -0.5),
                                                                     scalar2=None, op0=ALU.mult), reads=[P], writes=[qaT])
                yield 2.0
                for hf in range(2):
                    P = PF if hf == 0 else PSC
                    for hh in range(4):
                        h = hf * 4 + hh
                        for rc in range(2):
                            K.op("pe", lambda e, h=h, hh=hh, rc=rc, P=P: e.matmul(P[0:IDXD, hh * 128:(hh + 1) * 128],
                                                                                lhsT=wqi[:, rc, h * IDXD:(h + 1) * IDXD], rhs=cqT[:, rc, :],
                                                                                start=(rc == 0), stop=(rc == 1)), reads=[wqi, cqT], writes=[P])
                    K.op("act", lambda e, hf=hf, P=P: e.activation(out=qiT[:, :, hf * 4:(hf + 1) * 4].rearrange("p t h -> p h t"),
                                                                   in_=P[0:IDXD, 0:512].rearrange("p (h t) -> p h t", h=4), func=AF.Copy),
                         reads=[P], writes=[qiT])
                yield 2.0
                K.op("dve", lambda e: e.tensor_tensor(
                    out=Amat[:], in0=c.ident[:].rearrange("p (t o) -> p t o", o=1).to_broadcast([128, 128, H]),
                    in1=wi[:].rearrange("p (o h) -> p o h", o=1).to_broadcast([128, 128, H]), op=ALU.mult),
                     reads=[c.ident, wi], writes=[Amat])
                transposes(K, c, [Amat[:, g * 16:(g + 1) * 16, :].rearrange("p t h -> p (t h)") for g in range(8)], [Amat],
                           PF, Wg[:], Wg, 128, 128, copy_eng="dve")
                yield 3.0
                S = nsb_of(j) * 128
                nch = (S + 511) // 512
                for sc_ in range(nch):
                    n = min(512, S - sc_ * 512)
                    kc_ = kidc[sc_ % 2]
                    K.dma("sp", kc_[:, 0:n], kidxT_d.h[:, sc_ * 512:sc_ * 512 + n], reads=[kidxT_d], writes=[kc_])
                    for g in range(8):
                        K.op("pe", lambda e, g=g: e.matmul(PF[:, 0:n], lhsT=qiT[:, g * 16:(g + 1) * 16, :].rearrange("p t h -> p (t h)"),
                                                          rhs=kc_[:, 0:n], start=True, stop=True), reads=[qiT, kc_], writes=[PF])
                        Rt = Rsb[g % 2]
                        K.op("act", lambda e: e.activation(out=Rt[:, 0:n], in_=PF[:, 0:n], func=AF.Relu), reads=[PF], writes=[Rt])
                        K.op("pe", lambda e, g=g: e.matmul(PSC[:, 0:n], lhsT=Wg[:, g, :], rhs=Rt[:, 0:n],
                                                          start=(g == 0), stop=(g == 7)), reads=[Wg, Rt], writes=[PSC])
                        if g % 4 == 3:
                            yield 4.0 * n / 512
                    K.op("act", lambda e: e.activation(out=score[:, sc_ * 512:sc_ * 512 + n], in_=PSC[:, 0:n], func=AF.Copy),
                         reads=[PSC], writes=[score])

            def f1_cost(j):
                S = nsb_of(j) * 128
                return 15.0 + 2.0 * S / 512 * 4

            def f2(j):
                score, thr, msk = scores[j % 2], thrs[j % 2], mstage
                S = nsb_of(j) * 128
                if S <= TOPK:
                    K.op("dve", lambda e: e.tensor_tensor(out=score[:, S - CW:S], in0=score[:, S - CW:S], in1=cmask[:],
                                                          op=ALU.add), reads=[score, cmask], writes=[score])
                    K.op("dve", lambda e: e.memset(thr[:], -1e29), writes=[thr])
                    yield 1.0
                else:
                    K.op("dve", lambda e: e.tensor_reduce(out=lo[:], in_=score[:, 0:S], axis=AX.X, op=ALU.min),
                         reads=[score], writes=[lo])
                    K.op("dve", lambda e: e.tensor_tensor(out=score[:, S - CW:S], in0=score[:, S - CW:S], in1=cmask[:],
                                                          op=ALU.add), reads=[score, cmask], writes=[score])
                    yield S / 960.0
                    K.op("dve", lambda e: e.tensor_reduce(out=hi[:], in_=score[:, 0:S], axis=AX.X, op=ALU.max),
                         reads=[score], writes=[hi])
                    K.op("dve", lambda e: e.tensor_tensor(out=hi[:], in0=hi[:], in1=lo[:], op=ALU.subtract),
                         reads=[hi, lo], writes=[hi])
                    K.op("dve", lambda e: e.tensor_scalar(out=wtab[:], in0=p2[:], scalar1=hi[:], scalar2=None, op0=ALU.mult),
                         reads=[p2, hi], writes=[wtab])
                    K.op("dve", lambda e: e.tensor_tensor(out=mid[:], in0=lo[:], in1=wtab[:, 0:1], op=ALU.add),
                         reads=[lo, wtab], writes=[mid])
                    yield S / 960.0
                    for it in range(NIT):
                        K.op("dve", lambda e: e.tensor_scalar(out=msk[:, 0:S], in0=score[:, 0:S], scalar1=mid[:], scalar2=None,
                                                              op0=ALU.is_ge, op1=ALU.add, accum_out=cnt[:]),
                             reads=[score, mid], writes=[msk, cnt])
                        K.op("dve", lambda e: e.tensor_scalar(out=tsel[:], in0=cnt[:], scalar1=float(TOPK), scalar2=0.5,
                                                              op0=ALU.is_ge, op1=ALU.subtract), reads=[cnt], writes=[tsel])
                        K.op("dve", lambda e, it=it: e.scalar_tensor_tensor(out=mid[:], in0=tsel[:], scalar=wtab[:, it:it + 1],
                                                                            in1=mid[:], op0=ALU.mult, op1=ALU.add),
                             reads=[tsel, wtab, mid], writes=[mid])
                        yield S / 960.0 + 0.5
                    K.op("dve", lambda e: e.tensor_tensor(out=thr[:], in0=mid[:], in1=wtab[:, NIT:NIT + 1], op=ALU.subtract),
                         reads=[mid, wtab], writes=[thr])
                K.op("dve", lambda e: e.tensor_scalar(out=msk[:, 0:S], in0=score[:, 0:S], scalar1=thr[:], scalar2=None, op0=ALU.is_ge),
                     reads=[score, thr], writes=[msk])
                md = maskD[j % 2]
                K.dma("pool", md.h[:, 0:S], msk[:, 0:S], reads=[msk], writes=[md])
                yield S / 960.0

            def f2_cost(j):
                S = nsb_of(j) * 128
                return (NIT + 3) * (S / 960.0 + 0.5)

            def back(j):
                qaT = qaTs[j % 3]

                def maskfn(k, slot, mchunk, wi):
                    pm = c.PM[:].bitcast(BF16)[:, slot * 128:(slot + 1) * 128]
                    K.op("pe", lambda e: e.transpose(out=pm, in_=mchunk[:, wi * 128:(wi + 1) * 128], identity=c.ident[:]),
                         reads=[mchunk, c.ident], writes=[c.PM])
                    mt = mkT[slot]
                    K.op("act", lambda e: e.activation(out=mt[:], in_=pm, func=AF.Copy), reads=[c.PM], writes=[mt])
                    return mt[:].rearrange("p (g o q) -> p g o q", g=1, o=1).to_broadcast([128, 1, H, 128]), mt

                if allm:
                    tab_of = lambda k: min(j - k, NTAB - 1) * 128
                else:
                    tab_of = lambda k: min(2 * j - k + 1, NTAB - 1) * 128
                yield from attention_h(K, c, 1, H, qaT, list(range(nsb_of(j))), ckvT_d, ckva_d, ftab, tab_of, maskfn, mult_eng="pool",
                                       mask_dram=maskD[j % 2])

                def put(h, o_ap, rz_ap):
                    if h % 2 == 0:
                        K.op("act", lambda e: e.activation(out=olat[:, h, :], in_=o_ap, func=AF.Copy, scale=rz_ap),
                             reads=[c.PO, c.rz], writes=[olat])
                    else:
                        K.op("dve", lambda e: e.tensor_scalar(out=olat[:, h, :], in0=o_ap, scalar1=rz_ap, scalar2=None,
                                                              op0=ALU.mult), reads=[c.PO, c.rz], writes=[olat])
                attn_norm(K, c, H, put)
                yield 4.0
                transposes(K, c, [olat[:, h, :] for h in range(H)], [olat], c.Lh[0], olatT[:], olatT, 128, 128)
                for hf in range(2):
                    P = c.Lh[1] if hf == 0 else c.Lh[0]
                    for hh in range(4):
                        h = hf * 4 + hh
                        K.op("pe", lambda e, h=h, hh=hh, P=P: e.matmul(P[:, hh * 128:(hh + 1) * 128], lhsT=wuv[:, h * 128:(h + 1) * 128],
                                                                     rhs=olatT[:, h, :], start=True, stop=True), reads=[wuv, olatT], writes=[P])
                    K.op("act", lambda e, hf=hf, P=P: e.activation(out=oTs[:, hf * 4:(hf + 1) * 4, :], in_=P[:, 0:512], func=AF.Copy),
                         reads=[P], writes=[oTs])
                K.dma("pool", oT_d.h[:, :, j * 128:(j + 1) * 128], oTs[:], reads=[oTs], writes=[oT_d])
                yield 4.0

            def back_cost(j):
                return 2.4 * nsb_of(j) + 8.0

            def run_all(g):
                for _ in g:
                    pass

            for t in range(NQ + 2):
                streams = []
                if t < NQ:
                    streams.append([f1(t), f1_cost(t), 0.0])
                if 0 <= t - 1 < NQ:
                    streams.append([f2(t - 1), f2_cost(t - 1), 0.0])
                if 0 <= t - 2 < NQ:
                    streams.append([back(t - 2), back_cost(t - 2), 0.0])
                while streams:
                    st = min(streams, key=lambda s_: s_[2] / s_[1])
                    try:
                        st[2] += next(st[0])
                    except StopIteration:
                        streams.remove(st)
            K.barrier()
            K.es = saved

        with ExitStack() as es3:
            saved = K.es
            K.es = es3
            mlp_phase(K, c, xq, oT_d, wo_bf, wup_bf, wdn_bf, g_mlp, x1, NQ * 128)
            if debug:
                for nm, t, shp in (("ckva", ckva_d, [T, 130]), ("ckvT", ckvT_d, [128, T]), ("kidxT", kidxT_d, [IDXD, T]),
                                   ("oT", oT_d, [128 * H, NQ * 128])):
                    o = K.dram("dbg_" + nm, shp, F32, kind="ExternalOutput")
                    K.dma("pool", o.h, t.h.tensor.reshape(shp).ap(), reads=[t], writes=[o])
            K.barrier()
            K.es = saved
    return x1


CMP_LEN = 32
CMP_STRIDE = 16
CMP_HID = 256
N_SEL = 16


def build_B(T, debug=False):
    nc = bass.Bass("TRN2", target_bir_lowering=False)
    K = KB(nc)
    c = Ctx()
    with K.es:
        common_setup(K, c, T, T // 256)
        emit_B(K, c, T, None, debug)
        K.finish()
    return nc


def build_F(T):
    nc = bass.Bass("TRN2", target_bir_lowering=False)
    K = KB(nc)
    c = Ctx()
    with K.es:
        common_setup(K, c, T, T // 256)
        x1_all = emit_A(K, c, T, "all")
        emit_B(K, c, T, x1_all)
        K.finish()
    return nc


def emit_B(K, c, T, x1_src=None, debug=False):
    fused = x1_src is not None
    NQ = T // 256
    NS = T // 128
    NTAB = min(26, 2 * NQ)
    NB = T // 64
    NCP = T // 16
    n_cmp = NCP - 1
    NCC = NCP // 128
    ein = lambda n, s: K.dram(n, s, F32, kind="ExternalInput")
    if fused:
        xs = x1_src
        xq = None
        psel_d = ein("psel", [128, 2])
    else:
        xs = ein("xs", [T, D])
        xq = ein("xq", [NQ * 128, D])
    g_kvs = ein("g_kv_shared", [D])
    w_kvs = ein("w_kv_shared", [D, 1536])
    pos_k = ein("cmp_pos_k", [CMP_LEN, HD])
    pos_v = ein("cmp_pos_v", [CMP_LEN, HD])
    w1k = ein("cmp_w1_k", [CMP_LEN * HD, CMP_HID])
    w1v = ein("cmp_w1_v", [CMP_LEN * HD, CMP_HID])
    w2k = ein("cmp_w2_k", [CMP_HID, HD])
    w2v = ein("cmp_w2_v", [CMP_HID, HD])
    b_w_in = ein("b_w_in", [D, 1048])
    b_w_o = ein("b_w_o", [D, D])
    g_attn = ein("g_attn1", [D])
    g_mlp = ein("g_mlp1", [D])
    g_fin = ein("g_final", [D])
    w_up = ein("w_up1", [D, DFF])
    w_dn = ein("w_down1", [DFF, D])
    ftab_f = ein("ftabB", [128, H * NTAB * 128])
    wtab_f = ein("wtab", [128, H * 6 * 128])
    cm01_f = ein("cm01", [NQ * 128, NCC * 128])
    keep_d = ein("keep", [NQ * 128, NB])
    add_d = ein("addm", [NQ * 128, NB])
    selmap_f = ein("selmap", [NCP, NB])
    eall_f = ein("eall", [128, T])
    out = K.dram("out", [NQ * 128, D], F32, kind="ExternalOutput")

    sc = lambda n, s, dt=BF16: K.dram("B_" + n, s, dt)
    wkv_bf = sc("wkv_bf", [D, 1536])
    w1k_bf = sc("w1k_bf", [CMP_LEN * HD, CMP_HID])
    w1v_bf = sc("w1v_bf", [CMP_LEN * HD, CMP_HID])
    w2k_bf = sc("w2k_bf", [CMP_HID, HD])
    w2v_bf = sc("w2v_bf", [CMP_HID, HD])
    posk_bf = sc("posk_bf", [CMP_LEN, HD])
    posv_bf = sc("posv_bf", [CMP_LEN, HD])
    win_bf = sc("bwin_bf", [D, 1048])
    wo_bf = sc("wo_bf", [D, D])
    wup_bf = sc("wup_bf", [D, DFF])
    wdn_bf = sc("wdn_bf", [DFF, D])
    ftab_bf = sc("ftab_bf", [128, H * NTAB * 128])
    wtab_bf = sc("wtab_bf", [128, H * 6 * 128])
    cm01_bf = sc("cm01_bf", [NQ * 128, NCC * 128])
    selmap_bf = sc("selmap_bf", [NCP, NB])
    eall_bf = sc("eall_bf", [128, T])
    kslcT_d = sc("kslcT_d", [2, 128, T])
    kwinT_d = sc("kwinT_d", [2, 128, T])
    vslc_d = sc("vslc_d", [T, 2 * 130])
    vwin_d = sc("vwin_d", [T, 2 * 130])
    kcT_d = sc("kcT_d", [128, 2 * NCP])
    vca_d = sc("vca_d", [128, NCC * 2 * 130])
    oT_d = sc("oT_d", [128, H, NQ * 128])

    if True:
        for dst, src, n in ((wkv_bf, w_kvs, D * 1536), (w1k_bf, w1k, 4096 * 256), (w1v_bf, w1v, 4096 * 256),
                            (w2k_bf, w2k, 256 * 128), (w2v_bf, w2v, 256 * 128), (posk_bf, pos_k, 32 * 128), (posv_bf, pos_v, 32 * 128),
                            (win_bf, b_w_in, D * 1048), (ftab_bf, ftab_f, 128 * H * NTAB * 128), (wtab_bf, wtab_f, 128 * H * 6 * 128),
                            (cm01_bf, cm01_f, NQ * 128 * NCC * 128), (selmap_bf, selmap_f, NCP * NB), (eall_bf, eall_f, 128 * T),
                            (wo_bf, b_w_o, D * D), (wup_bf, w_up, D * DFF), (wdn_bf, w_dn, DFF * D)):
            cast_to_scratch(K, dst, src, n)

        with ExitStack() as es1:
            saved = K.es
            K.es = es1
            gk = K.sb("gk", [128, D], F32)
            load_bcast(K, gk, g_kvs, D)
            wkv = K.sb("wkv", [128, 8, 1536], BF16)
            K.dma("sp", wkv[:], wkv_bf.h.rearrange("(k p) n -> p k n", p=128), reads=[wkv_bf], writes=[wkv])
            cmpT = K.sb("cmpT", [128, 4, T], BF16)
            xb = [K.sb("xb", [128, D], F32) for _ in range(2)]
            hb = K.sb("hb1", [128, D], BF16)
            hT4 = K.sb("hT4", [128, 8, 512], BF16)
            stg = [K.sb("stg", [128, 512], BF16) for _ in range(2)]
            vst = [K.sb("vst", [128, 2, 2, 130], BF16) for _ in range(2)]
            for t in vst:
                K.op("dve", lambda e, t=t: e.memset(t[:, :, :, 128:129], 1.0), writes=[t])
                K.op("dve", lambda e, t=t: e.memset(t[:, :, :, 129:130], 0.0), writes=[t])
            fm_cols = [(0, "c", 0), (128, "c", 1), (256, "c", 2), (384, "c", 3), (512, "s", 0), (640, "s", 1), (1024, "w", 0), (1152, "w", 1)]
            cnt_stg = 0
            cnt_v = 0
            hT4s = [hT4, K.sb("hT4b", [128, 8, 512], BF16)]

            def b1_stage1(tg):
                for sub in range(4):
                    i = tg * 4 + sub
                    x = xb[i % 2]
                    K.dma("sp", x[:], xs.h[i * 128:(i + 1) * 128, :], reads=[xs], writes=[x])
                    rmsnorm_rows(K, c, x[:], x, D, gk[:], gk, hb[:], hb, "a")
                    transposes(K, c, [hb[:, k * 128:(k + 1) * 128] for k in range(8)], [hb], c.PM,
                               hT4s[tg % 2][:, :, sub * 128:(sub + 1) * 128], hT4s[tg % 2], 128, 128, copy_eng="act" if sub % 2 == 0 else "dve")

            b1_stage1(0)
            for tg in range(T // 512):
                if tg + 1 < T // 512:
                    b1_stage1(tg + 1)
                hT4 = hT4s[tg % 2]
                for ci, (col0, kind, idx) in enumerate(fm_cols):
                    P = c.PL[ci % 2]
                    for k in range(8):
                        K.op("pe", lambda e, k=k, P=P, col0=col0: e.matmul(P[:, 0:512], lhsT=wkv[:, k, col0:col0 + 128], rhs=hT4[:, k, :],
                                                                           start=(k == 0), stop=(k == 7)), reads=[wkv, hT4], writes=[P])
                    if kind == "c":
                        K.op("act", lambda e, P=P, idx=idx: e.activation(out=cmpT[:, idx, tg * 512:(tg + 1) * 512], in_=P[:, 0:512], func=AF.Copy),
                             reads=[P], writes=[cmpT])
                    else:
                        st = stg[cnt_stg % 2]
                        cnt_stg += 1
                        K.op("dve", lambda e, P=P, st=st: e.tensor_copy(out=st[:], in_=P[:, 0:512]), reads=[P], writes=[st])
                        dst = kslcT_d if kind == "s" else kwinT_d
                        K.dma("pool", dst.h[idx, :, tg * 512:(tg + 1) * 512], st[:], reads=[st], writes=[dst])
                for sub in range(4):
                    i = tg * 4 + sub
                    vt = vst[cnt_v % 2]
                    cnt_v += 1
                    P = c.PL[sub % 2]
                    for br, col0 in enumerate((768, 1280)):
                        for k in range(8):
                            K.op("pe", lambda e, k=k, P=P, col0=col0, br=br: e.matmul(
                                P[:, br * 256:(br + 1) * 256], lhsT=hT4[:, k, sub * 128:(sub + 1) * 128], rhs=wkv[:, k, col0:col0 + 256],
                                start=(k == 0), stop=(k == 7)), reads=[hT4, wkv], writes=[P])
                    K.op("act", lambda e, P=P, vt=vt: e.activation(
                        out=vt[:, :, :, 0:128], in_=P[:, 0:512].rearrange("p (b g d) -> p b g d", b=2, g=2), func=AF.Copy),
                         reads=[P], writes=[vt])
                    K.dma("pool", vslc_d.h[i * 128:(i + 1) * 128, :], vt[:, 0, :, :].rearrange("p g d -> p (g d)"), reads=[vt], writes=[vslc_d])
                    K.dma("pool", vwin_d.h[i * 128:(i + 1) * 128, :], vt[:, 1, :, :].rearrange("p g d -> p (g d)"), reads=[vt], writes=[vwin_d])
            w1 = K.sb("w1", [128, CMP_LEN, CMP_HID], BF16)
            w2 = K.sb("w2", [128, 2, HD], BF16)
            posr = K.sb("posr", [CMP_LEN, HD], BF16)
            posT = K.sb("posT", [128, CMP_LEN], BF16)
            c0 = K.sb("c0", [128, 2], F32)
            hid = K.sb("hid", [128, 2, NCP], BF16)
            K.op("dve", lambda e: e.memset(hid[:], 0.0), writes=[hid])
            u = K.sb("u", [128, NCP], F32)
            t1 = K.sb("t1", [128, NCP], F32)
            kcT = K.sb("kcT", [128, 2, NCP], BF16)
            K.op("dve", lambda e: e.memset(kcT[:], 0.0), writes=[kcT])
            vca = K.sb("vca", [128, NCC, 2, 130], BF16)
            K.op("dve", lambda e: e.memset(vca[:], 0.0), writes=[vca])
            K.op("dve", lambda e: e.memset(vca[:, :, :, 128:129], 1.0), writes=[vca])
            for kv, (w1_bf, w2_bf, pos_bf) in enumerate(((w1k_bf, w2k_bf, posk_bf), (w1v_bf, w2v_bf, posv_bf))):
                K.dma("sp", w1[:], w1_bf.h.rearrange("(l p) n -> p l n", p=128), reads=[w1_bf], writes=[w1])
                K.dma("sp", w2[:], w2_bf.h.rearrange("(k p) n -> p k n", p=128), reads=[w2_bf], writes=[w2])
                K.dma("sp", posr[:], pos_bf.h, reads=[pos_bf], writes=[posr])
                transposes(K, c, [posr[:, :]], [posr], c.PM, posT[:], posT, 128, CMP_LEN)
                for hc in range(2):
                    for l in range(CMP_LEN):
                        K.op("pe", lambda e, l=l, hc=hc: e.matmul(c.PM[:, hc * 2:hc * 2 + 1], lhsT=w1[:, l, hc * 128:(hc + 1) * 128], rhs=posT[:, l:l + 1],
                                                                  start=(l == 0), stop=(l == CMP_LEN - 1)), reads=[w1, posT], writes=[c.PM])
                    K.op("dve", lambda e, hc=hc: e.tensor_copy(out=c0[:, hc:hc + 1], in_=c.PM[:, hc * 2:hc * 2 + 1]), reads=[c.PM], writes=[c0])
                for g in range(2):
                    src = cmpT[:, kv * 2 + g, :]
                    for hc in range(2):
                        P = c.PL[hc]
                        for l in range(CMP_LEN):
                            rhs = bass.AP(tensor=src.tensor, offset=src.offset + l, ap=[list(src.ap[0]), [CMP_STRIDE, n_cmp]])
                            K.op("pe", lambda e, l=l, hc=hc, rhs=rhs, P=P: e.matmul(P[:, 0:n_cmp], lhsT=w1[:, l, hc * 128:(hc + 1) * 128], rhs=rhs,
                                                                                    start=(l == 0), stop=(l == CMP_LEN - 1)), reads=[w1, cmpT], writes=[P])
                        K.op("act", lambda e, hc=hc, P=P: e.activation(out=u[:, 0:n_cmp], in_=P[:, 0:n_cmp], func=AF.Identity, bias=c0[:, hc:hc + 1]),
                             reads=[P, c0], writes=[u])
                        K.op("dve", lambda e: e.tensor_tensor(out=t1[:, 0:n_cmp], in0=u[:, 0:n_cmp], in1=u[:, 0:n_cmp], op=ALU.mult), reads=[u], writes=[t1])
                        K.op("dve", lambda e: e.tensor_scalar(out=t1[:, 0:n_cmp], in0=t1[:, 0:n_cmp], scalar1=0.044715, scalar2=1.0, op0=ALU.mult, op1=ALU.add),
                             reads=[t1], writes=[t1])
                        K.op("dve", lambda e: e.tensor_tensor(out=t1[:, 0:n_cmp], in0=t1[:, 0:n_cmp], in1=u[:, 0:n_cmp], op=ALU.mult), reads=[t1, u], writes=[t1])
                        K.op("act", lambda e: e.activation(out=t1[:, 0:n_cmp], in_=t1[:, 0:n_cmp], func=AF.Tanh, scale=float(math.sqrt(2.0 / math.pi))),
                             reads=[t1], writes=[t1])
                        K.op("dve", lambda e: e.tensor_scalar(out=t1[:, 0:n_cmp], in0=t1[:, 0:n_cmp], scalar1=0.5, scalar2=0.5, op0=ALU.mult, op1=ALU.add),
                             reads=[t1], writes=[t1])
                        K.op("dve", lambda e, hc=hc: e.tensor_tensor(out=hid[:, hc, 0:n_cmp], in0=t1[:, 0:n_cmp], in1=u[:, 0:n_cmp], op=ALU.mult),
                             reads=[t1, u], writes=[hid])
                    if kv == 0:
                        P = c.PL[0]
                        for hc in range(2):
                            K.op("pe", lambda e, hc=hc, P=P: e.matmul(P[:, 0:n_cmp], lhsT=w2[:, hc, :], rhs=hid[:, hc, 0:n_cmp],
                                                                      start=(hc == 0), stop=(hc == 1)), reads=[w2, hid], writes=[P])
                        K.op("act", lambda e, g=g, P=P: e.activation(out=kcT[:, g, 0:n_cmp], in_=P[:, 0:n_cmp], func=AF.Copy), reads=[P], writes=[kcT])
                    else:
                        for cc in range(NCC):
                            P = c.PL[cc % 2]
                            for hc in range(2):
                                K.op("pe", lambda e, hc=hc, P=P, cc=cc: e.matmul(P[:, 0:128], lhsT=hid[:, hc, cc * 128:(cc + 1) * 128], rhs=w2[:, hc, :],
                                                                                 start=(hc == 0), stop=(hc == 1)), reads=[w2, hid], writes=[P])
                            K.op("act", lambda e, g=g, P=P, cc=cc: e.activation(out=vca[:, cc, g, 0:128], in_=P[:, 0:128], func=AF.Copy),
                                 reads=[P], writes=[vca])
            K.dma("pool", kcT_d.h, kcT[:].rearrange("p g n -> p (g n)"), reads=[kcT], writes=[kcT_d])
            K.dma("pool", vca_d.h, vca[:].rearrange("p c g d -> p (c g d)"), reads=[vca], writes=[vca_d])
            K.barrier()
            K.es = saved

        with ExitStack() as es2:
            saved = K.es
            K.es = es2
            ga = K.sb("ga", [128, D], F32)
            load_bcast(K, ga, g_attn, D)
            wbin = K.sb("wbin", [128, 8, 1048], BF16)
            K.dma("sp", wbin[:], win_bf.h.rearrange("(k p) n -> p k n", p=128), reads=[win_bf], writes=[wbin])
            ftab = K.sb("ftab", [128, H, NTAB * 128], BF16)
            K.dma("sp", ftab[:], ftab_bf.h.rearrange("p (h n) -> p h n", h=H), reads=[ftab_bf], writes=[ftab])
            wtab = K.sb("wtab", [128, H, 6 * 128], BF16)
            K.dma("sp", wtab[:], wtab_bf.h.rearrange("p (h n) -> p h n", h=H), reads=[wtab_bf], writes=[wtab])
            kcT = K.sb("kcT", [128, 2, NCP], BF16)
            K.dma("sp", kcT[:], kcT_d.h.rearrange("p (g n) -> p g n", g=2), reads=[kcT_d], writes=[kcT])
            vca = K.sb("vca", [128, NCC, 2, 130], BF16)
            K.dma("sp", vca[:], vca_d.h.rearrange("p (c g d) -> p c g d", c=NCC, g=2), reads=[vca_d], writes=[vca])
            selmap = K.sb("selmap", [128, NCC, NB], BF16)
            K.dma("sp", selmap[:], selmap_bf.h.rearrange("(c p) j -> p c j", p=128), reads=[selmap_bf], writes=[selmap])
            eall = K.sb("eall", [128, T], BF16)
            K.dma("sp", eall[:], eall_bf.h, reads=[eall_bf], writes=[eall])
            xb = [K.sb("xb", [128, D], F32) for _ in range(2)]
            if fused:
                xp2 = [K.sb("xp2", [128, 2, D], F32) for _ in range(2)]
                psel = K.sb("psel", [128, 2], F32)
                K.dma("sp", psel[:], psel_d.h, reads=[psel_d], writes=[psel])
            hb = K.sb("hb2", [128, D], BF16)
            hT = K.sb("hT2", [128, 8, 128], BF16)
            qT = K.sb("qT", [128, H * 128], BF16)
            gates = K.sb("gates", [128, 24], F32)
            scg = K.sb("scg", [128, 8], F32)
            oacc = K.sb("oacc", [128, D], F32)
            oab = K.sb("oab", [128, D], BF16)
            oTs = K.sb("oTs", [128, H, 128], BF16)
            imp = K.sb("imp", [128, 2, NB], F32)
            imt = K.sb("imt", [128, NB], F32)
            m8a = K.sb("m8a", [128, 8], F32)
            m8b = K.sb("m8b", [128, 8], F32)
            selm = K.sb("selm", [128, 2, 128], BF16)
            K.op("dve", lambda e: e.memset(selm[:], 0.0), writes=[selm])
            selT = K.sb("selT", [128, 2, 128], BF16)
            keep = [K.sb("keep", [128, NB], F32) for _ in range(2)]
            addm = [K.sb("addm", [128, NB], F32) for _ in range(2)]
            cm01 = [K.sb("cm01", [128, NCC, 128], BF16) for _ in range(2)]
            PTc = [K.sb("PTc", [128, 512], BF16) for _ in range(2)]
            c.kTc = [K.sb("kTc", [128, 2, 512], BF16) for _ in range(2)]
            c.vc = [K.sb("vc", [128, 4, 260], BF16) for _ in range(2)]
            c.PT = [K.sb("PT", [128, 1024], BF16) for _ in range(2)]
            c.rz = K.sb("rz", [128, 8], F32)

            for j in range(NQ):
                x = xb[j % 2]
                if fused:
                    xp = xp2[j % 2]
                    K.dma("sp", xp[:], xs.h[2 * j * 128:(2 * j + 2) * 128, :].rearrange("(s p) n -> p s n", p=128), reads=[xs], writes=[xp])
                    K.op("dve", lambda e: e.tensor_scalar(out=x[:], in0=xp[:, 0, :], scalar1=psel[:, 0:1], scalar2=None, op0=ALU.mult),
                         reads=[xp, psel], writes=[x])
                    K.op("dve", lambda e: e.scalar_tensor_tensor(out=x[:], in0=xp[:, 1, :], scalar=psel[:, 1:2], in1=x[:], op0=ALU.mult, op1=ALU.add),
                         reads=[xp, psel, x], writes=[x])
                else:
                    K.dma("sp", x[:], xq.h[j * 128:(j + 1) * 128, :], reads=[xq], writes=[x])
                kp, am, cm = keep[j % 2], addm[j % 2], cm01[j % 2]
                K.dma("sp", kp[:], keep_d.h[j * 128:(j + 1) * 128, :], reads=[keep_d], writes=[kp])
                K.dma("sp", am[:], add_d.h[j * 128:(j + 1) * 128, :], reads=[add_d], writes=[am])
                K.dma("sp", cm[:], cm01_bf.h[j * 128:(j + 1) * 128, :].rearrange("p (c q) -> p c q", c=NCC), reads=[cm01_bf], writes=[cm])
                rmsnorm_rows(K, c, x[:], x, D, ga[:], ga, hb[:], hb, "a")
                transposes(K, c, [hb[:, k * 128:(k + 1) * 128] for k in range(8)], [hb], c.PM, hT[:], hT, 128, 128)
                P0 = c.PL[0]
                for h in range(H):
                    for k in range(8):
                        K.op("pe", lambda e, h=h, k=k: e.matmul(P0[:, h * 128:(h + 1) * 128], lhsT=wbin[:, k, h * 128:(h + 1) * 128], rhs=hT[:, k, :],
                                                               start=(k == 0), stop=(k == 7)), reads=[wbin, hT], writes=[P0])
                K.op("dve", lambda e: e.tensor_scalar(out=qT[:], in0=P0[:], scalar1=float(HD ** -0.5), scalar2=None, op0=ALU.mult),
                     reads=[P0], writes=[qT])
                P1 = c.PL[1]
                for k in range(8):
                    K.op("pe", lambda e, k=k: e.matmul(P1[:, 0:24], lhsT=hT[:, k, :], rhs=wbin[:, k, 1024:1048], start=(k == 0), stop=(k == 7)),
                         reads=[hT, wbin], writes=[P1])
                K.op("act", lambda e: e.activation(out=gates[:], in_=P1[:, 0:24], func=AF.Sigmoid), reads=[P1], writes=[gates])

                for g in range(2):
                    for cc in range(NCC):
                        L = c.PL[cc % 2]
                        K.op("pe", lambda e, L=L, cc=cc: e.matmul(L[:, 0:512], lhsT=kcT[:, g, cc * 128:(cc + 1) * 128], rhs=qT[:, g * 512:(g + 1) * 512],
                                                                  start=True, stop=True), reads=[kcT, qT], writes=[L])
                        pt = PTc[cc % 2]
                        K.op("act", lambda e, L=L, pt=pt: e.activation(out=pt[:], in_=L[:, 0:512], func=AF.Exp), reads=[L], writes=[pt])
                        pv = pt[:].rearrange("p (r q) -> p r q", r=4)
                        mv = cm[:, cc, :].rearrange("p (o q) -> p o q", o=1).to_broadcast([128, 4, 128])
                        K.op("dve", lambda e, pv=pv, mv=mv: e.tensor_tensor(out=pv, in0=pv, in1=mv, op=ALU.mult), reads=[pt, cm], writes=[pt])
                        for r in range(4):
                            bank, slot = divmod(r, 3)
                            K.op("pe", lambda e, r=r, bank=bank, slot=slot, pt=pt, cc=cc: e.matmul(
                                c.PO[:, bank, slot * 130:(slot + 1) * 130], lhsT=pt[:, r * 128:(r + 1) * 128], rhs=vca[:, cc, g, :],
                                start=(cc == 0 and slot == 0), stop=(cc == NCC - 1), skip_group_check=True), reads=[pt, vca], writes=[c.PO])
                        for r in range(4):
                            K.op("pe", lambda e, r=r, pt=pt, cc=cc: e.matmul(
                                c.PO[:, 2, r * NB:(r + 1) * NB], lhsT=pt[:, r * 128:(r + 1) * 128], rhs=selmap[:, cc, :],
                                start=(cc == 0 and r == 0), stop=(cc == NCC - 1), skip_group_check=True), reads=[pt, selmap], writes=[c.PO])
                    for bank, n in ((0, 3), (1, 1)):
                        zs = c.PO[:, bank, :]
                        zsrc = bass.AP(tensor=zs.tensor, offset=zs.offset + 128, ap=[list(zs.ap[0]), [130, n], [1, 1]])
                        K.op("dve", lambda e, zsrc=zsrc, bank=bank, n=n: e.tensor_scalar(
                            out=c.rz[:, 3 * bank:3 * bank + n].rearrange("p (n o) -> p n o", o=1), in0=zsrc, scalar1=1e-30, scalar2=None,
                            op0=ALU.max), reads=[c.PO], writes=[c.rz])
                    K.op("dve", lambda e: e.reciprocal(out=c.rz[:, 0:4], in_=c.rz[:, 0:4]), reads=[c.rz], writes=[c.rz])
                    gv = bass.AP(tensor=gates[:].tensor, offset=gates[:].offset + g * 12, ap=[list(gates[:].ap[0]), [3, 4]])
                    K.op("dve", lambda e, gv=gv: e.tensor_tensor(out=scg[:, 0:4], in0=c.rz[:, 0:4], in1=gv, op=ALU.mult),
                         reads=[c.rz, gates], writes=[scg])
                    for r in range(4):
                        bank, slot = divmod(r, 3)
                        h = g * 4 + r
                        K.op("act" if r % 2 == 0 else "dve",
                             (lambda e, h=h, bank=bank, slot=slot, r=r: e.activation(out=oacc[:, h * 128:(h + 1) * 128], in_=c.PO[:, bank, slot * 130:slot * 130 + 128],
                                                                                    func=AF.Copy, scale=scg[:, r:r + 1])) if r % 2 == 0 else
                             (lambda e, h=h, bank=bank, slot=slot, r=r: e.tensor_scalar(out=oacc[:, h * 128:(h + 1) * 128], in0=c.PO[:, bank, slot * 130:slot * 130 + 128],
                                                                                       scalar1=scg[:, r:r + 1], scalar2=None, op0=ALU.mult)),
                             reads=[c.PO, scg], writes=[oacc])
                    K.op("dve", lambda e, g=g: e.tensor_scalar(out=imp[:, g, :], in0=c.PO[:, 2, 0:NB], scalar1=c.rz[:, 0:1], scalar2=None, op0=ALU.mult),
                         reads=[c.PO, c.rz], writes=[imp])
                    for r in range(1, 4):
                        K.op("dve", lambda e, g=g, r=r: e.scalar_tensor_tensor(out=imp[:, g, :], in0=c.PO[:, 2, r * NB:(r + 1) * NB], scalar=c.rz[:, r:r + 1],
                                                                               in1=imp[:, g, :], op0=ALU.mult, op1=ALU.add), reads=[c.PO, c.rz, imp], writes=[imp])
                for g in range(2):
                    K.op("dve", lambda e, g=g: e.tensor_tensor(out=imp[:, g, :], in0=imp[:, g, :], in1=kp[:], op=ALU.mult), reads=[imp, kp], writes=[imp])
                    K.op("dve", lambda e, g=g: e.tensor_tensor(out=imp[:, g, :], in0=imp[:, g, :], in1=am[:], op=ALU.add), reads=[imp, am], writes=[imp])
                    K.op("dve", lambda e, g=g: e.max(out=m8a[:], in_=imp[:, g, :]), reads=[imp], writes=[m8a])
                    K.op("dve", lambda e, g=g: e.match_replace(out=imt[:], in_to_replace=m8a[:], in_values=imp[:, g, :], imm_value=-3.0e38),
                         reads=[imp, m8a], writes=[imt])
                    K.op("dve", lambda e: e.max(out=m8b[:], in_=imt[:]), reads=[imt], writes=[m8b])
                    K.op("dve", lambda e, g=g: e.tensor_scalar(out=selm[:, g, 0:NB], in0=imp[:, g, :], scalar1=m8b[:, 7:8], scalar2=None, op0=ALU.is_ge),
                         reads=[imp, m8b], writes=[selm])
                transposes(K, c, [selm[:, 0, :], selm[:, 1, :]], [selm], c.PM, selT[:], selT, 128, 128, copy_eng="dve")

                def maskfn(k, slot):
                    pm = c.PM[:, slot * 256:(slot + 1) * 256]
                    K.op("pe", lambda e: e.matmul(pm, lhsT=eall[:, k * 128:(k + 1) * 128], rhs=selT[:].rearrange("p g q -> p (g q)"),
                                                  start=True, stop=True), reads=[eall, selT], writes=[c.PM])
                    return pm.rearrange("p (g o q) -> p g o q", g=2, o=1).to_broadcast([128, 2, 4, 128]), c.PM

                def make_put(br, first=False):
                    def gate_fn():
                        gv = bass.AP(tensor=gates[:].tensor, offset=gates[:].offset + br, ap=[list(gates[:].ap[0]), [3, 8]])
                        K.op("dve", lambda e: e.tensor_tensor(out=scg[:], in0=c.rz[:], in1=gv, op=ALU.mult), reads=[c.rz, gates], writes=[scg])

                    def put(h, o_ap, rz_ap):
                        K.op("dve", lambda e: e.scalar_tensor_tensor(out=oacc[:, h * 128:(h + 1) * 128], in0=o_ap, scalar=scg[:, h:h + 1],
                                                                     in1=oacc[:, h * 128:(h + 1) * 128], op0=ALU.mult, op1=ALU.add),
                             reads=[c.PO, scg, oacc], writes=[oacc])
                    return put, gate_fn

                attention(K, c, 2, 4, qT, list(range(2 * j + 2)), kslcT_d, vslc_d, ftab,
                          lambda k: min(2 * j - k + 1, NTAB - 1) * 128, maskfn)
                put, gate_fn = make_put(1)
                attn_norm(K, c, H, put, gate_fn)
                attention(K, c, 2, 4, qT, list(range(max(0, 2 * j - 4), 2 * j + 2)), kwinT_d, vwin_d, wtab,
                          lambda k: (2 * j - k + 1) * 128, None)
                put, gate_fn = make_put(2)
                attn_norm(K, c, H, put, gate_fn)
                K.op("act", lambda e: e.activation(out=oab[:], in_=oacc[:], func=AF.Copy), reads=[oacc], writes=[oab])
                transposes(K, c, [oab[:, h * 128:(h + 1) * 128] for h in range(H)], [oab], c.PM, oTs[:], oTs, 128, 128)
                K.dma("pool", oT_d.h[:, :, j * 128:(j + 1) * 128], oTs[:], reads=[oTs], writes=[oT_d])
            K.barrier()
            K.es = saved

        with ExitStack() as es3:
            saved = K.es
            K.es = es3
            mlp_phase(K, c, xs if fused else xq, oT_d, wo_bf, wup_bf, wdn_bf, g_mlp, out, NQ * 128, final_g=g_fin,
                      blend=psel_d if fused else None)
            if debug:
                for nm, t, shp in (("kslcT", kslcT_d, [256, T]), ("vslc", vslc_d, [T, 260]), ("kcT", kcT_d, [128, 2 * NCP]),
                                   ("vca", vca_d, [128, NCC * 260]), ("oT", oT_d, [128 * H, NQ * 128]), ("kwinT", kwinT_d, [256, T]), ("vwin", vwin_d, [T, 260])):
                    o = K.dram("dbg_" + nm, shp, F32, kind="ExternalOutput")
                    K.dma("pool", o.h, t.h.tensor.reshape(shp).ap(), reads=[t], writes=[o])
            K.barrier()
            K.es = saved


def _rel_bucket(dist):
    dist = jnp.maximum(dist, 0)
    exact = 16
    log_ratio = jnp.log(jnp.maximum(dist, 1).astype(jnp.float32) / exact) / math.log(4096 / exact)
    large = exact + (log_ratio * (32 - exact)).astype(jnp.int32)
    return jnp.where(dist < exact, dist, jnp.minimum(large, 31))


def make_ftab(rel_bias, p, ntab, lo_valid=0, hi_valid=None):
    s = np.arange(128)[:, None]
    col = np.arange(ntab * 128)[None, :]
    dist = col - 128 + 128 * p - s
    with jax.default_device(jax.devices("cpu")[0]):
        b = np.asarray(_rel_bucket(jnp.asarray(dist, dtype=jnp.int32)))
    tab = np.asarray(rel_bias, dtype=np.float32)[b]
    ok = dist >= lo_valid
    if hi_valid is not None:
        ok &= dist < hi_valid
    tab = np.where(ok[:, :, None], tab, np.float32(NEGB))
    return np.ascontiguousarray(np.transpose(tab, (0, 2, 1))).reshape(128, -1).astype(np.float32)


_CACHE = {}


def run_A(T, x, inp, debug=False):
    B = x.shape[0]
    NQ = T // 256
    NTAB = min(26, 2 * NQ)
    if ("A", T, debug) not in _CACHE:
        _CACHE[("A", T, debug)] = build_A(T, debug)
    nc = _CACHE[("A", T, debug)]
    f = lambda a: np.ascontiguousarray(np.asarray(a, dtype=np.float32))
    in_maps = []
    for core in range(N_CORES):
        b, p = core // 2, core % 2
        xb = x[b % B]
        xq = xb.reshape(T // 128, 128, D)[p::2].reshape(NQ * 128, D)
        q = np.arange(128)[:, None]
        cc = np.arange(256)[None, :]
        cmask = np.where(cc > 128 * p + q, np.float32(-1e30), np.float32(0.0)).astype(np.float32)
        in_maps.append({
            "xs": f(xb), "xq": f(xq),
            "a_w_in": f(inp["a_w_in"][0]), "g_attn0": f(inp["g_attn"][0]), "g_mlp0": f(inp["g_mlp"][0]),
            "a_g_q_lat": f(inp["a_g_q_lat"][0]), "a_g_kv_lat": f(inp["a_g_kv_lat"][0]), "a_g_k_idx": f(inp["a_g_k_idx"][0]),
            "a_w_uq": f(inp["a_w_uq"][0]).reshape(QL, H * HD), "a_w_q_idx": f(inp["a_w_q_idx"][0]).reshape(QL, H * IDXD),
            "a_w_uk": f(inp["a_w_uk"][0]).reshape(KVL, H * HD), "a_w_uv": f(inp["a_w_uv"][0]).reshape(KVL, H * HD),
            "a_w_o": f(inp["a_w_o"][0]), "w_up0": f(inp["w_up"][0]), "w_down0": f(inp["w_down"][0]),
            "ftabA": make_ftab(inp["rel_bias"], p, NTAB), "cmask": cmask,
        })
    res = run_bass_kernel_spmd(nc, in_maps, core_ids=list(range(N_CORES)))
    x1 = np.zeros((B, T, D), np.float32)
    for core in range(N_CORES):
        b, p = core // 2, core % 2
        if b < B:
            x1[b].reshape(T // 128, 128, D)[p::2] = res.results[core]["x1"].reshape(NQ, 128, D)
    if debug:
        return x1, res.results
    return x1


def _sel_map(n_cmp, n_slc):
    c0 = CMP_STRIDE * np.arange(n_cmp)[:, None]
    s0 = 64 * np.arange(n_slc)[None, :]
    ov = np.clip(np.minimum(c0 + CMP_LEN, s0 + 64) - np.maximum(c0, s0), 0, None)
    return (ov / CMP_LEN).astype(np.float32)


def b_static_inputs(T, p, inp):
    NQ = T // 256
    NTAB = min(26, 2 * NQ)
    NB = T // 64
    NCP = T // 16
    n_cmp = NCP - 1
    NCC = NCP // 128
    f = lambda a: np.ascontiguousarray(np.asarray(a, dtype=np.float32))
    selmap = np.zeros((NCP, NB), np.float32)
    selmap[:n_cmp] = _sel_map(n_cmp, NB)
    eall = np.zeros((128, T), np.float32)
    eall[np.arange(T) // 64, np.arange(T)] = 1.0
    t = (128 * (2 * np.arange(NQ)[:, None] + p) + np.arange(128)[None, :]).reshape(-1)
    n = np.arange(NCP)
    cm = ((CMP_STRIDE * n[None, :] + CMP_LEN - 1 <= t[:, None]) & (n[None, :] < n_cmp)).astype(np.float32)
    cm01 = cm.reshape(NQ, 128, NCC, 128).transpose(0, 3, 2, 1).reshape(NQ * 128, NCC * 128)
    blk = np.arange(NB)[None, :]
    cur = (t // 64)[:, None]
    forced = (blk == 0) | (blk == cur) | (blk == cur - 1)
    future = blk * 64 > t[:, None]
    keep = (~(forced | future)).astype(np.float32)
    addm = np.where(future, np.float32(-1e30), np.where(forced, np.float32(1e9), np.float32(0.0))).astype(np.float32)
    return {
        "g_kv_shared": f(inp["g_kv_shared"]), "w_kv_shared": f(inp["w_kv_shared"]),
        "cmp_pos_k": f(inp["cmp_pos_k"]), "cmp_pos_v": f(inp["cmp_pos_v"]),
        "cmp_w1_k": f(inp["cmp_w1_k"]), "cmp_w1_v": f(inp["cmp_w1_v"]), "cmp_w2_k": f(inp["cmp_w2_k"]), "cmp_w2_v": f(inp["cmp_w2_v"]),
        "b_w_in": f(inp["b_w_in"][0]), "b_w_o": f(inp["b_w_o"][0]),
        "g_attn1": f(inp["g_attn"][1]), "g_mlp1": f(inp["g_mlp"][1]), "g_final": f(inp["g_final"]),
        "w_up1": f(inp["w_up"][1]), "w_down1": f(inp["w_down"][1]),
        "ftabB": make_ftab(inp["rel_bias"], p, NTAB), "wtab": make_ftab(inp["rel_bias"], p, 6, 0, 512),
        "cm01": f(cm01), "keep": f(keep), "addm": f(addm), "selmap": selmap, "eall": eall,
    }


def a_static_inputs(T, inp):
    f = lambda a: np.ascontiguousarray(np.asarray(a, dtype=np.float32))
    return {
        "a_w_in": f(inp["a_w_in"][0]), "g_attn0": f(inp["g_attn"][0]), "g_mlp0": f(inp["g_mlp"][0]),
        "a_g_q_lat": f(inp["a_g_q_lat"][0]), "a_g_kv_lat": f(inp["a_g_kv_lat"][0]), "a_g_k_idx": f(inp["a_g_k_idx"][0]),
        "a_w_uq": f(inp["a_w_uq"][0]).reshape(QL, H * HD), "a_w_q_idx": f(inp["a_w_q_idx"][0]).reshape(QL, H * IDXD),
        "a_w_uk": f(inp["a_w_uk"][0]).reshape(KVL, H * HD), "a_w_uv": f(inp["a_w_uv"][0]).reshape(KVL, H * HD),
        "a_w_o": f(inp["a_w_o"][0]), "w_up0": f(inp["w_up"][0]), "w_down0": f(inp["w_down"][0]),
    }


def run_B(T, x1, inp, debug=False):
    B = x1.shape[0]
    NQ = T // 256
    if ("B", T, debug) not in _CACHE:
        _CACHE[("B", T, debug)] = build_B(T, debug)
    nc = _CACHE[("B", T, debug)]
    f = lambda a: np.ascontiguousarray(np.asarray(a, dtype=np.float32))
    in_maps = []
    for core in range(N_CORES):
        b, p = core // 2, core % 2
        xb = x1[b % B]
        xq = xb.reshape(T // 128, 128, D)[p::2].reshape(NQ * 128, D)
        m = {"xs": f(xb), "xq": f(xq)}
        m.update(b_static_inputs(T, p, inp))
        in_maps.append(m)
    res = run_bass_kernel_spmd(nc, in_maps, core_ids=list(range(N_CORES)))
    out = np.zeros((B, T, D), np.float32)
    for core in range(N_CORES):
        b, p = core // 2, core % 2
        if b < B:
            out[b].reshape(T // 128, 128, D)[p::2] = res.results[core]["out"].reshape(NQ, 128, D)
    if debug:
        return out, res.results
    return out


def run_F(T, x, inp):
    B = x.shape[0]
    NQ = T // 256
    NS = T // 128
    if ("F", T) not in _CACHE:
        _CACHE[("F", T)] = build_F(T)
    nc = _CACHE[("F", T)]
    f = lambda a: np.ascontiguousarray(np.asarray(a, dtype=np.float32))
    a_st = a_static_inputs(T, inp)
    q = np.arange(128)[:, None]
    cc = np.arange(128)[None, :]
    cmask = np.where(cc > q, np.float32(-1e30), np.float32(0.0)).astype(np.float32)
    ftabA = make_ftab(inp["rel_bias"], 1, min(26, NS))
    in_maps = []
    for core in range(N_CORES):
        b, p = core // 2, core % 2
        m = {"xs": f(x[b % B]), "ftabA": ftabA, "cmask": cmask,
             "psel": np.ascontiguousarray(np.broadcast_to(np.array([1.0 - p, float(p)], np.float32), (128, 2)))}
        m.update(a_st)
        m.update(b_static_inputs(T, p, inp))
        in_maps.append(m)
    res = run_bass_kernel_spmd(nc, in_maps, core_ids=list(range(N_CORES)))
    out = np.zeros((B, T, D), np.float32)
    for core in range(N_CORES):
        b, p = core // 2, core % 2
        if b < B:
            out[b].reshape(T // 128, 128, D)[p::2] = res.results[core]["out"].reshape(NQ, 128, D)
    return out


def kernel(**inputs):
    x = np.asarray(inputs["x"], dtype=np.float32)
    B, T, _ = x.shape
    return run_F(T, x, inputs)
```
